# Optimizing a Trainium2 kernel written in Bass

```python
import math
import jax, jax.numpy as jnp
from jax import lax
import numpy as np


D_MODEL = 1024
BATCH = 32
SEQ = 2048
DEPTH = 2

CTX_LEN = 256
GRID_W = 64
N_MIXERS = 2
N_GLA = (DEPTH + N_MIXERS - 1) // N_MIXERS
N_MLA = DEPTH // N_MIXERS
ALPHA = (2.0 * DEPTH) ** 0.25
BETA = (8.0 * DEPTH) ** -0.25
LN_EPS = 1e-5
RMS_EPS = 1e-6

GLA_HEADS = 4
GLA_DK = D_MODEL // 2 // GLA_HEADS
GLA_DV = D_MODEL // GLA_HEADS
GLA_GATE_RANK = 16
GLA_TAU = 16.0
GLA_CHUNK = 64

MLA_HEADS = D_MODEL // 128
MLA_NOPE = 128
MLA_ROPE = 64
MLA_V = 128
MLA_Q_RANK = 256
MLA_KV_RANK = 128
ATTN_BLOCK = 128
ROPE_BASE = 10000.0

PEER_HEADS = 8
PEER_NKEYS = 128
PEER_EXPERTS = PEER_NKEYS * PEER_NKEYS
PEER_QDIM = 256
PEER_TOPK = 16
PEER_BLOCK = 128

kernel_name = 'hybrid_gla_mla_peer_diffusion_block'


def layer_norm(x, g, b):
    xf = x.astype(jnp.float32)
    mu = jnp.mean(xf, axis=-1, keepdims=True)
    var = jnp.mean(jnp.square(xf - mu), axis=-1, keepdims=True)
    return ((xf - mu) * lax.rsqrt(var + LN_EPS)).astype(x.dtype) * g + b


def rms_norm(x, g):
    xf = x.astype(jnp.float32)
    return (xf * lax.rsqrt(jnp.mean(xf * xf, axis=-1, keepdims=True) + RMS_EPS)).astype(x.dtype) * g


def modulate(x, shift, scale):
    return x * (1 + scale) + shift


def axial_rope_tables(n_tokens):
    rows = n_tokens // GRID_W
    row = jnp.repeat(jnp.arange(rows, dtype=jnp.float32), GRID_W)
    col = jnp.tile(jnp.arange(GRID_W, dtype=jnp.float32), rows)
    n_freq = MLA_ROPE // 4
    inv_freq = ROPE_BASE ** (-jnp.arange(n_freq, dtype=jnp.float32) / n_freq)
    ang = jnp.stack([row[:, None] * inv_freq, col[:, None] * inv_freq], axis=1)
    return jnp.cos(ang), jnp.sin(ang)


def rope_2d(x, cos, sin):
    xs = x.reshape(x.shape[:-1] + (2, 2, MLA_ROPE // 4))
    cos = cos.astype(x.dtype)
    sin = sin.astype(x.dtype)
    x1, x2 = xs[..., 0, :], xs[..., 1, :]
    out = jnp.stack([x1 * cos - x2 * sin, x1 * sin + x2 * cos], axis=-2)
    return out.reshape(x.shape)


def gla_chunked(q, k, v, logg, s0, strict):
    b_, h_, l_, dk = q.shape
    dv = v.shape[-1]
    n = l_ // GLA_CHUNK
    rs = lambda t: t.reshape(b_, h_, n, GLA_CHUNK, t.shape[-1])
    q, k, v, logg = rs(q), rs(k), rs(v), rs(logg)
    cum = jnp.cumsum(logg.astype(jnp.float32), axis=3)
    last = cum[..., -1:, :]
    qe = q * jnp.exp(cum).astype(q.dtype)
    ke = k * jnp.exp(-cum).astype(k.dtype)
    kd = k * jnp.exp(last - cum).astype(k.dtype)
    i = jnp.arange(GLA_CHUNK)
    mask = (i[:, None] > i[None, :]) if strict else (i[:, None] >= i[None, :])
    a = jnp.where(mask, jnp.einsum('bhnid,bhnjd->bhnij', qe, ke), 0)
    intra = jnp.einsum('bhnij,bhnjv->bhniv', a, v)
    chunk_kv = jnp.einsum('bhnjd,bhnjv->bhndv', kd, v)
    decay = jnp.exp(last[..., 0, :]).astype(v.dtype)

    def step(s, xs):
        qe_n, kv_n, dec_n = xs
        out = jnp.einsum('bhid,bhdv->bhiv', qe_n, s)
        return dec_n[..., None] * s + kv_n, out

    s_final, inter = lax.scan(step, s0, (jnp.moveaxis(qe, 2, 0), jnp.moveaxis(chunk_kv, 2, 0), jnp.moveaxis(decay, 2, 0)))
    o = intra + jnp.moveaxis(inter, 0, 2)
    return o.reshape(b_, h_, l_, dv), s_final


def gla_mixer(x_lat, x_ctx, w_in, gf_a, gf_b, gf_bias, gb_a, gb_b, gb_bias, norm_g, w_out, need_ctx):
    hk, hv = GLA_HEADS * GLA_DK, GLA_HEADS * GLA_DV

    def heads(t, d):
        b_, l_, _ = t.shape
        return t.reshape(b_, l_, GLA_HEADS, d).transpose(0, 2, 1, 3)

    def project(x):
        q, k, v, r = jnp.split(x @ w_in, [hk, 2 * hk, 2 * hk + hv], axis=-1)
        lf = jax.nn.log_sigmoid(x @ gf_a @ gf_b + gf_bias) / GLA_TAU
        lb = jax.nn.log_sigmoid(x @ gb_a @ gb_b + gb_bias) / GLA_TAU
        return (heads(q, GLA_DK) * GLA_DK ** -0.5, heads(k, GLA_DK), heads(v, GLA_DV), r,
                heads(lf, GLA_DK), heads(lb, GLA_DK))

    ql, kl, vl, rl, lfl, lbl = project(x_lat)
    qc, kc, vc, rc, lfc, lbc = project(x_ctx)
    flip = lambda t: jnp.flip(t, axis=2)
    s0 = jnp.zeros(qc.shape[:2] + (GLA_DK, GLA_DV), qc.dtype)
    oc_f, sc_f = gla_chunked(qc, kc, vc, lfc, s0, False)
    oc_b, sc_b = gla_chunked(flip(qc), flip(kc), flip(vc), flip(lbc), s0, True)
    ol_f, _ = gla_chunked(ql, kl, vl, lfl, sc_f, False)
    ol_b, _ = gla_chunked(flip(ql), flip(kl), flip(vl), flip(lbl), sc_b, True)

    def output(o, r):
        o = rms_norm(o, norm_g)
        b_, h_, l_, dv = o.shape
        o = o.transpose(0, 2, 1, 3).reshape(b_, l_, h_ * dv)
        return (o * jax.nn.silu(r)) @ w_out

    out_lat = output(ol_f + flip(ol_b), rl)
    out_ctx = output(oc_f + flip(oc_b), rc) if need_ctx else None
    return out_lat, out_ctx


def softmax_attend(q, k, v):
    s = jnp.einsum('bqhd,bkhd->bhqk', q, k) * (MLA_NOPE + MLA_ROPE) ** -0.5
    p = jax.nn.softmax(s.astype(jnp.float32), axis=-1).astype(v.dtype)
    return jnp.einsum('bhqk,bkhd->bqhd', p, v)


def mla_mixer(x_lat, x_ctx, cos, sin, w_down, q_norm_g, kv_norm_g, w_uq, w_ukv, w_out, need_ctx):
    def project(x, rotate):
        b_, l_, _ = x.shape
        cq, ckv, kr = jnp.split(x @ w_down, [MLA_Q_RANK, MLA_Q_RANK + MLA_KV_RANK], axis=-1)
        q = (rms_norm(cq, q_norm_g) @ w_uq).reshape(b_, l_, MLA_HEADS, MLA_NOPE + MLA_ROPE)
        kv = (rms_norm(ckv, kv_norm_g) @ w_ukv).reshape(b_, l_, MLA_HEADS, MLA_NOPE + MLA_V)
        q_nope, q_rope = q[..., :MLA_NOPE], q[..., MLA_NOPE:]
        k_nope, v = kv[..., :MLA_NOPE], kv[..., MLA_NOPE:]
        if rotate:
            q_rope = rope_2d(q_rope, cos[:, None], sin[:, None])
            kr = rope_2d(kr, cos, sin)
        k_rope = jnp.broadcast_to(kr[:, :, None, :], (b_, l_, MLA_HEADS, MLA_ROPE))
        return jnp.concatenate([q_nope, q_rope], -1), jnp.concatenate([k_nope, k_rope], -1), v

    q_l, k_l, v_l = project(x_lat, True)
    q_c, k_c, v_c = project(x_ctx, False)
    b_, l_ = x_lat.shape[:2]
    k_all = jnp.concatenate([k_c, k_l], axis=1)
    v_all = jnp.concatenate([v_c, v_l], axis=1)
    n_blk = l_ // ATTN_BLOCK
    q_blocks = q_l.reshape(b_, n_blk, ATTN_BLOCK, MLA_HEADS, MLA_NOPE + MLA_ROPE).swapaxes(0, 1)
    o_l = lax.map(lambda qb: softmax_attend(qb, k_all, v_all), q_blocks)
    o_l = o_l.swapaxes(0, 1).reshape(b_, l_, MLA_HEADS * MLA_V)
    out_lat = o_l @ w_out
    if need_ctx:
        o_c = softmax_attend(q_c, k_c, v_c)
        out_ctx = o_c.reshape(o_c.shape[0], o_c.shape[1], MLA_HEADS * MLA_V) @ w_out
    else:
        out_ctx = None
    return out_lat, out_ctx


def peer(x, w_query, keys_a, keys_b, expert_u, expert_v):
    t = x.shape[0]
    xb = x.reshape(t // PEER_BLOCK, PEER_BLOCK, D_MODEL)

    def block(xt):
        q = (xt @ w_query).reshape(PEER_BLOCK, PEER_HEADS, 2, PEER_QDIM // 2)
        s_a = jnp.einsum('thd,hkd->thk', q[:, :, 0], keys_a)
        s_b = jnp.einsum('thd,hkd->thk', q[:, :, 1], keys_b)
        va, ia = lax.top_k(s_a, PEER_TOPK)
        vb, ib = lax.top_k(s_b, PEER_TOPK)
        cand = (va[..., :, None] + vb[..., None, :]).reshape(PEER_BLOCK, PEER_HEADS, PEER_TOPK * PEER_TOPK)
        vals, pos = lax.top_k(cand, PEER_TOPK)
        idx = (jnp.take_along_axis(ia, pos // PEER_TOPK, axis=-1) * PEER_NKEYS
               + jnp.take_along_axis(ib, pos % PEER_TOPK, axis=-1))
        g = jax.nn.softmax(vals.astype(jnp.float32), axis=-1).astype(xt.dtype)
        idx = idx.reshape(PEER_BLOCK, PEER_HEADS * PEER_TOPK)
        h = jax.nn.gelu(jnp.einsum('td,ted->te', xt, expert_u[idx]), approximate=False)
        w = g.reshape(PEER_BLOCK, PEER_HEADS * PEER_TOPK) * h
        return jnp.einsum('te,ted->td', w, expert_v[idx])

    return lax.map(block, xb).reshape(t, D_MODEL)


def setup_inputs(seed: int = 0) -> dict:
    key = jax.random.key(seed)
    ks = jax.random.split(key, 32)
    nrm = lambda k, shape, s: jax.random.normal(k, shape, jnp.float32) * s
    hk, hv = GLA_HEADS * GLA_DK, GLA_HEADS * GLA_DV
    d = D_MODEL
    return {
        'x': nrm(ks[0], (BATCH, SEQ, d), 1.0),
        'c': nrm(ks[1], (BATCH, d), 1.0),
        'ctx': nrm(ks[2], (BATCH, CTX_LEN, d), 1.0),
        'c_ctx': nrm(ks[3], (d,), 1.0),
        'ada_w': nrm(ks[4], (DEPTH, d, 6 * d), 0.5 * d ** -0.5),
        'ada_b': nrm(ks[5], (DEPTH, 6 * d), 0.01),
        'ln_tm_g': 1.0 + nrm(ks[6], (DEPTH, d), 0.02),
        'ln_tm_b': nrm(ks[7], (DEPTH, d), 0.01),
        'ln_cm_g': 1.0 + nrm(ks[8], (DEPTH, d), 0.02),
        'ln_cm_b': nrm(ks[9], (DEPTH, d), 0.01),
        'gla_w_in': nrm(ks[10], (N_GLA, d, 2 * hk + 2 * hv), d ** -0.5),
        'gla_gate_fwd_a': nrm(ks[11], (N_GLA, d, GLA_GATE_RANK), d ** -0.5),
        'gla_gate_fwd_b': nrm(ks[12], (N_GLA, GLA_GATE_RANK, hk), GLA_GATE_RANK ** -0.5),
        'gla_gate_fwd_bias': nrm(ks[13], (N_GLA, hk), 0.1),
        'gla_gate_bwd_a': nrm(ks[14], (N_GLA, d, GLA_GATE_RANK), d ** -0.5),
        'gla_gate_bwd_b': nrm(ks[15], (N_GLA, GLA_GATE_RANK, hk), GLA_GATE_RANK ** -0.5),
        'gla_gate_bwd_bias': nrm(ks[16], (N_GLA, hk), 0.1),
        'gla_norm_g': 1.0 + nrm(ks[17], (N_GLA, GLA_DV), 0.02),
        'gla_w_out': nrm(ks[18], (N_GLA, hv, d), BETA * hv ** -0.5),
        'mla_w_down': nrm(ks[19], (N_MLA, d, MLA_Q_RANK + MLA_KV_RANK + MLA_ROPE), d ** -0.5),
        'mla_q_norm_g': 1.0 + nrm(ks[20], (N_MLA, MLA_Q_RANK), 0.02),
        'mla_kv_norm_g': 1.0 + nrm(ks[21], (N_MLA, MLA_KV_RANK), 0.02),
        'mla_w_uq': nrm(ks[22], (N_MLA, MLA_Q_RANK, MLA_HEADS * (MLA_NOPE + MLA_ROPE)), MLA_Q_RANK ** -0.5),
        'mla_w_ukv': nrm(ks[23], (N_MLA, MLA_KV_RANK, MLA_HEADS * (MLA_NOPE + MLA_V)), MLA_KV_RANK ** -0.5),
        'mla_w_out': nrm(ks[24], (N_MLA, MLA_HEADS * MLA_V, d), BETA * (MLA_HEADS * MLA_V) ** -0.5),
        'peer_w_query': nrm(ks[25], (DEPTH, d, PEER_HEADS * PEER_QDIM), d ** -0.5),
        'peer_keys_a': nrm(ks[26], (DEPTH, PEER_HEADS, PEER_NKEYS, PEER_QDIM // 2), (PEER_QDIM // 2) ** -0.5),
        'peer_keys_b': nrm(ks[27], (DEPTH, PEER_HEADS, PEER_NKEYS, PEER_QDIM // 2), (PEER_QDIM // 2) ** -0.5),
        'peer_u': nrm(ks[28], (DEPTH, PEER_EXPERTS, d), d ** -0.5),
        'peer_v': nrm(ks[29], (DEPTH, PEER_EXPERTS, d), BETA),
    }


def reference(x, c, ctx, c_ctx, ada_w, ada_b, ln_tm_g, ln_tm_b, ln_cm_g, ln_cm_b,
              gla_w_in, gla_gate_fwd_a, gla_gate_fwd_b, gla_gate_fwd_bias,
              gla_gate_bwd_a, gla_gate_bwd_b, gla_gate_bwd_bias, gla_norm_g, gla_w_out,
              mla_w_down, mla_q_norm_g, mla_kv_norm_g, mla_w_uq, mla_w_ukv, mla_w_out,
              peer_w_query, peer_keys_a, peer_keys_b, peer_u, peer_v):
    b_, l_, d = x.shape
    cos, sin = axial_rope_tables(l_)
    h_lat, h_ctx = x, ctx
    sc_silu = jax.nn.silu(c)
    cc_silu = jax.nn.silu(c_ctx)
    for i in range(DEPTH):
        last = i == DEPTH - 1
        mod = sc_silu @ ada_w[i] + ada_b[i]
        mod_c = cc_silu @ ada_w[i] + ada_b[i]
        sh_t, sc_t, g_t, sh_c, sc_c, g_c = jnp.split(mod[:, None, :], 6, axis=-1)
        csh_t, csc_t, cg_t, csh_c, csc_c, cg_c = jnp.split(mod_c, 6, axis=-1)
        xl = modulate(h_lat, sh_t, sc_t)
        xc = modulate(h_ctx, csh_t, csc_t)
        j = i // N_MIXERS
        if i % N_MIXERS == 0:
            out_l, out_c = gla_mixer(xl, xc, gla_w_in[j], gla_gate_fwd_a[j], gla_gate_fwd_b[j], gla_gate_fwd_bias[j],
                                     gla_gate_bwd_a[j], gla_gate_bwd_b[j], gla_gate_bwd_bias[j],
                                     gla_norm_g[j], gla_w_out[j], not last)
        else:
            out_l, out_c = mla_mixer(xl, xc, cos, sin, mla_w_down[j], mla_q_norm_g[j], mla_kv_norm_g[j],
                                     mla_w_uq[j], mla_w_ukv[j], mla_w_out[j], not last)
        h_lat = layer_norm(ALPHA * h_lat + g_t * out_l, ln_tm_g[i], ln_tm_b[i])
        xl = modulate(h_lat, sh_c, sc_c).reshape(b_ * l_, d)
        if not last:
            h_ctx = layer_norm(ALPHA * h_ctx + cg_t * out_c, ln_tm_g[i], ln_tm_b[i])
            xc = modulate(h_ctx, csh_c, csc_c).reshape(-1, d)
            y = peer(jnp.concatenate([xl, xc], axis=0), peer_w_query[i], peer_keys_a[i], peer_keys_b[i], peer_u[i], peer_v[i])
            y_l, y_c = y[:b_ * l_], y[b_ * l_:]
            h_ctx = layer_norm(ALPHA * h_ctx + cg_c * y_c.reshape(h_ctx.shape), ln_cm_g[i], ln_cm_b[i])
        else:
            y_l = peer(xl, peer_w_query[i], peer_keys_a[i], peer_keys_b[i], peer_u[i], peer_v[i])
        h_lat = layer_norm(ALPHA * h_lat + g_c * y_l.reshape(b_, l_, d), ln_cm_g[i], ln_cm_b[i])
    return h_lat
```

```python
from contextlib import ExitStack
import numpy as np
import ml_dtypes
import concourse.bass as bass
import concourse.mybir as mybir
from concourse.bass_utils import run_bass_kernel_spmd

F32 = mybir.dt.float32
BF16 = mybir.dt.bfloat16
AF = mybir.ActivationFunctionType
ALU = mybir.AluOpType
AX = mybir.AxisListType

EPOCH = 30000
import os as _os
DBG = int(_os.environ.get('MLA_DBG', '0'))
ENGS = ['tensor', 'vector', 'scalar', 'gpsimd', 'sync']


class Prog:
    def __init__(self, n_dma_sems=64):
        self.nc = bass.Bass("TRN2", target_bir_lowering=False)
        self.stack = ExitStack()
        self.ops = {e: [] for e in ENGS}
        self.cnt = {e: 0 for e in ENGS}
        self.nsem = 0
        self.esem = {e: self._newsem() for e in ENGS}
        self.seen = {e: {} for e in ENGS}
        self.lastw = {}
        self.readers = {}
        self.dsem = [self._newsem() for _ in range(n_dma_sems)]
        self.dcnt = [0] * n_dma_sems
        self.n_hw = n_dma_sems - 16
        self.drr = {'hw': 0, 'sw': 0}
        self.n_inst = 0

    def _newsem(self):
        self.nsem += 1
        s = self.stack.enter_context(self.nc.semaphore("s%d" % self.nsem))
        return (self.nsem, s)

    def sb(self, name, shape, dt):
        return self.stack.enter_context(self.nc.sbuf_tensor(name, shape, dt))

    def ps(self, name, shape, dt):
        return self.stack.enter_context(self.nc.psum_tensor(name, shape, dt))

    def _deps(self, eng, reads, writes):
        toks = []
        for k in list(reads) + list(writes):
            t = self.lastw.get(k)
            if t is not None:
                toks.append(t)
        for k in writes:
            toks.extend(self.readers.get(k, ()))
        best = {}
        for (sid, sem, val, teng) in toks:
            if teng == eng and eng == 'tensor':
                continue
            if sid not in best or best[sid][1] < val:
                best[sid] = (sem, val)
        waits = []
        seen = self.seen[eng]
        for sid, (sem, val) in best.items():
            if seen.get(sid, 0) >= val:
                continue
            seen[sid] = val
            waits.append((sem, val))
        return waits

    def _record(self, tok, reads, writes):
        for k in reads:
            lst = self.readers.setdefault(k, [])
            if lst and lst[-1][0] == tok[0]:
                lst[-1] = tok
            else:
                lst.append(tok)
        for k in writes:
            self.lastw[k] = tok
            self.readers[k] = []

    def op(self, eng, fn, reads=(), writes=()):
        psr = [k for k in reads if isinstance(k, tuple) and k[0] == "ps"]
        if psr:
            reads = [k for k in reads if k not in psr]
            writes = list(writes) + psr
        waits = self._deps(eng, reads, writes)
        if self.cnt[eng] >= EPOCH:
            self.esem[eng] = self._newsem()
            self.cnt[eng] = 0
        self.cnt[eng] += 1
        sid, sem = self.esem[eng]
        tok = (sid, sem, self.cnt[eng], eng)
        self.ops[eng].append((waits, fn, sem, 1))
        self._record(tok, reads, writes)
        self.n_inst += 1

    def dma(self, eng, out, in_, reads=(), writes=(), **kw):
        if eng == 'gpsimd':
            i = self.n_hw + self.drr['sw']
            self.drr['sw'] = (self.drr['sw'] + 1) % 16
        else:
            i = self.drr['hw']
            self.drr['hw'] = (self.drr['hw'] + 1) % self.n_hw
        sid, sem = self.dsem[i]
        toks_extra = []
        if self.dcnt[i] > 0:
            toks_extra.append((sid, sem, self.dcnt[i], 'dma'))
        waits = self._deps(eng, reads, writes)
        seen = self.seen[eng]
        for (sid2, sem2, val, _) in toks_extra:
            if seen.get(sid2, 0) < val:
                seen[sid2] = val
                waits.append((sem2, val))
        self.dcnt[i] += 16
        tok = (sid, sem, self.dcnt[i], 'dma')
        self.ops[eng].append((waits, lambda e: e.dma_start(out=out, in_=in_, **kw), sem, 16))
        self._record(tok, reads, writes)
        self.n_inst += 1

    def finish(self, eng, keys):
        waits = self._deps(eng, keys, ())
        self.ops[eng].append((waits, None, None, 0))

    def barrier_keys(self):
        return list(self.lastw.keys())

    def emit(self):
        nc = self.nc
        ops = self.ops
        with nc.Block() as block:
            def replay(e, name):
                for (waits, fn, sem, inc) in ops[name]:
                    for (s, v) in waits:
                        e.wait_ge(s, v)
                    if fn is not None:
                        fn(e).then_inc(sem, inc)

            @block.tensor
            def _(e):
                replay(e, 'tensor')

            @block.vector
            def _(e):
                replay(e, 'vector')

            @block.scalar
            def _(e):
                replay(e, 'scalar')

            @block.gpsimd
            def _(e):
                replay(e, 'gpsimd')

            @block.sync
            def _(e):
                replay(e, 'sync')
        self.stack.close()

    def make_identity(self, ident):
        pass


def _mm(P, out, lhsT, rhs, start=True, stop=True, reads=(), writes=()):
    P.op('tensor', lambda e: e.matmul(out, lhsT=lhsT, rhs=rhs, start=start, stop=stop), reads, writes)


def _act(P, out, in_, func, reads=(), writes=(), **kw):
    P.op('scalar', lambda e: e.activation(out=out, in_=in_, func=func, **kw), reads, writes)


def _tt(P, eng, out, in0, in1, op, reads=(), writes=()):
    P.op(eng, lambda e: e.tensor_tensor(out=out, in0=in0, in1=in1, op=op), reads, writes)


def _ts(P, eng, out, in0, s1, s2, op0, op1=None, reads=(), writes=()):
    if op1 is None:
        P.op(eng, lambda e: e.tensor_scalar(out=out, in0=in0, scalar1=s1, scalar2=None, op0=op0), reads, writes)
    else:
        P.op(eng, lambda e: e.tensor_scalar(out=out, in0=in0, scalar1=s1, scalar2=s2, op0=op0, op1=op1), reads, writes)


def _stt(P, out, in0, scalar, in1, op0, op1, reads=(), writes=()):
    P.op('vector', lambda e: e.scalar_tensor_tensor(out=out, in0=in0, scalar=scalar, in1=in1, op0=op0, op1=op1), reads, writes)


def _cp(P, eng, out, in_, reads=(), writes=()):
    if eng == 'scalar':
        P.op(eng, lambda e: e.activation(out=out, in_=in_, func=AF.Copy), reads, writes)
    else:
        P.op(eng, lambda e: e.tensor_copy(out=out, in_=in_), reads, writes)


def _memset(P, eng, ap, val, reads=(), writes=()):
    P.op(eng, lambda e: e.memset(ap, val), reads, writes)


D = 1024
SEQ = 2048
CTX = 256
DEPTH = 2
ALPHA = (2.0 * DEPTH) ** 0.25
LN_EPS = 1e-5
RMS_EPS = 1e-6
NCORES = 8
LT = SEQ // 128
CT = CTX // 128
NEXP = 16384


def host_consts():
    c = {}
    c['ident'] = np.eye(128, dtype=np.float32).astype(ml_dtypes.bfloat16)
    tp = np.arange(128)[:, None]
    t = np.arange(128)[None, :]
    same = (tp // 64) == (t // 64)
    s = -1.0 / 16.0
    gm = np.zeros((128, 4, 128), np.float32)
    gm[:, 0] = (same & (tp <= t)) * s
    gm[:, 1] = (same & (tp > t)) * s
    gm[:, 2] = (same & (tp >= t)) * s
    gm[:, 3] = (same & (tp < t)) * s
    c['gmask'] = gm
    sel = np.zeros((128, 2), np.float32)
    sel[:64, 0] = s
    sel[64:, 1] = s
    c['selc'] = sel
    j = np.arange(128)[:, None]
    i = np.arange(128)[None, :]
    am = np.zeros((128, 2, 4, 128), np.float32)
    am[:, 0] = ((same & (i >= j)) * 1.0)[:, None, :]
    am[:, 1] = ((same & (j > i)) * 1.0)[:, None, :]
    c['amask'] = am
    row = np.repeat(np.arange(SEQ // 64, dtype=np.float32), 64)
    col = np.tile(np.arange(64, dtype=np.float32), SEQ // 64)
    inv = (10000.0 ** (-np.arange(16, dtype=np.float32) / 16)).astype(np.float32)
    ang = np.stack([row[:, None] * inv, col[:, None] * inv], axis=1)
    c['cos'] = np.cos(ang).astype(np.float32).reshape(SEQ, 32)
    c['sin'] = np.sin(ang).astype(np.float32).reshape(SEQ, 32)
    return c


class K:
    def __init__(self, NB, stages, ext=()):
        self.NB = NB
        self.P = P = Prog()
        self.nc = nc = P.nc
        self.ext = set(ext)
        self.stages = stages
        self.dr = {}
        self.psum = P.ps("psum", [128, 4096], F32)
        self.ident = P.sb("s_ident", [128, 128], BF16)
        self.pool_off = 0
        self.done = set()
        self.batches = list(range(NB))
        self.batch_outer = True

    def sbuf(self, st, n, s, d):
        self.uid = getattr(self, 'uid', 0) + 1
        return st.enter_context(self.nc.sbuf_tensor("s%d_%s" % (self.uid, n), s, d))

    def bank(self, b, c0=0, c1=512):
        return self.psum[:, b * 512 + c0: b * 512 + c1]

    def dram(self, name, shape, dt, kind=None):
        if name in self.dr:
            return self.dr[name]
        if kind is None:
            kind = "Internal"
            if name in self.ext:
                kind = "ExternalOutput"
        t = self.nc.dram_tensor(name, list(shape), dt, kind=kind).ap()
        self.dr[name] = t
        return t

    def inp(self, name, shape, dt=F32):
        return self.dram(name, shape, dt, kind="ExternalInput")

    def tiles(self, with_ctx=True):
        out = []
        for b in self.batches:
            if with_ctx:
                for j in range(CT):
                    out.append(('c', b, j))
            for j in range(LT):
                out.append(('l', b, j))
        return out

    def tid(self, t):
        kind, b, j = t
        return (b - self.batches[0]) * (CT + LT) + (j if kind == 'c' else CT + j)

    def rows(self, ap, t):
        i = self.tid(t)
        return ap[i * 128:(i + 1) * 128]

    def modrow(self, t):
        return 4 if t[0] == 'c' else t[1]

    def h_in(self, t):
        kind, b, j = t
        if kind == 'c':
            return self.ctx_d[b, j * 128:(j + 1) * 128, :]
        return self.x_d[b, j * 128:(j + 1) * 128, :]

    def barrier(self):
        P = self.P
        toks = []
        for e in ENGS:
            sid, sem = P.esem[e]
            if P.cnt[e] > 0:
                toks.append((sid, sem, P.cnt[e]))
        for i, (sid, sem) in enumerate(P.dsem):
            if P.dcnt[i] > 0:
                toks.append((sid, sem, P.dcnt[i]))
        for e in ENGS:
            waits = []
            for (sid, sem, val) in toks:
                if P.seen[e].get(sid, 0) < val:
                    P.seen[e][sid] = val
                    waits.append((sem, val))
            P.ops[e].append((waits, None, None, 0))
        P.lastw = {}
        P.readers = {}

    def load_cast(self, st, dst, src, ncols, tag, eng='gpsimd'):
        P = self.P
        stg = self.sbuf(st, "stg_" + tag, [128, 2, 2048], F32)
        n = 0
        for (d, s, w) in self._chunks(dst, src, ncols):
            par = n % 2
            P.dma('sync', stg[:, par, 0:w], s, writes=[("stg" + tag, par)])
            _cp(P, eng, d, stg[:, par, 0:w], reads=[("stg" + tag, par)], writes=[tag])
            n += 1

    def _chunks(self, dst, src, ncols):
        out = []
        for d, s in zip(dst, src):
            for c0 in range(0, ncols, 2048):
                w = min(2048, ncols - c0)
                out.append((d[:, c0:c0 + w], s[:, c0:c0 + w], w))
        return out

    def bcast_load(self, dst, row_ap, key, eng='sync'):
        self.P.dma(eng, dst, row_ap.partition_broadcast(128), writes=[key])

    def layernorm(self, st, z, out, g_t, b_t, keyz, keyout, reads_extra, tag):
        P = self.P
        tag = tag + "_%d" % id(st)
        if not hasattr(self, '_ln_' + tag):
            setattr(self, '_ln_' + tag, (
                self.sbuf(st, "lnst_" + tag, [128, 2, 6], F32),
                self.sbuf(st, "lnmv_" + tag, [128, 4], F32)))
        stt, mv = getattr(self, '_ln_' + tag)
        ks, km = "lnst_" + tag, "lnmv_" + tag
        for i in range(2):
            P.op('vector', lambda e, i=i: e.bn_stats(out=stt[:, i, :], in_=z[:, i * 512:(i + 1) * 512]), reads=[keyz], writes=[ks])
        P.op('vector', lambda e: e.bn_aggr(out=mv[:, 0:2], in_=stt[:].rearrange("p a b -> p (a b)")), reads=[ks], writes=[km])
        _ts(P, 'vector', mv[:, 2:3], mv[:, 1:2], LN_EPS, None, ALU.add, reads=[km], writes=[km])
        _act(P, mv[:, 2:3], mv[:, 2:3], AF.Sqrt, reads=[km], writes=[km])
        P.op('vector', lambda e: e.reciprocal(out=mv[:, 3:4], in_=mv[:, 2:3]), reads=[km], writes=[km])
        _ts(P, 'vector', z, z, mv[:, 0:1], mv[:, 3:4], ALU.subtract, ALU.mult, reads=[km, keyz], writes=[keyz])
        _tt(P, 'gpsimd', z, z, g_t, ALU.mult, reads=[keyz] + reads_extra, writes=[keyz])
        _tt(P, 'vector', out, z, b_t, ALU.add, reads=[keyz] + reads_extra, writes=[keyout])

    def stage_prep(self):
        P, nc = self.P, self.nc
        cT = self.inp("cT", [128, 8, 5])
        ada_w = self.inp("ada_w", [2, D, 6 * D])
        ada_b = self.inp("ada_b", [2, 6 * D])
        mod_d = self.dram("mod_d", [2, 5, 6 * D], F32)
        with ExitStack() as st:
            sb = lambda n, s, d: self.sbuf(st, n, s, d)
            cs = sb("cs", [128, 8, 5], F32)
            wt = sb("adaw", [128, 2, 8, 512], F32)
            bt = sb("adab", [5, 2, 512], F32)
            res = sb("adares", [5, 2, 512], F32)
            P.dma('sync', cs[:], cT[:, :, :], writes=["cs"])
            _act(P, cs[:], cs[:], AF.Silu, reads=["cs"], writes=["cs"])
            n = 0
            for l in range(2):
                wv = ada_w[l].rearrange("(kc p) n -> p kc n", p=128)
                for nb in range(12):
                    par = n % 2
                    P.dma('sync', wt[:, par], wv[:, :, nb * 512:(nb + 1) * 512], writes=[("adaw", par)])
                    P.dma('gpsimd', bt[:, par, :], ada_b[l, nb * 512:(nb + 1) * 512].partition_broadcast(5), writes=[("adab", par)])
                    for kc in range(8):
                        _mm(P, self.psum[0:5, par * 512:(par + 1) * 512], cs[:, kc, :], wt[:, par, kc, :], start=(kc == 0), stop=(kc == 7),
                            reads=["cs", ("adaw", par)], writes=[("ps", par)])
                    _tt(P, 'vector', res[:, par, :], self.psum[0:5, par * 512:(par + 1) * 512], bt[:, par, :], ALU.add,
                        reads=[("ps", par), ("adab", par)], writes=[("adares", par)])
                    P.dma('gpsimd', mod_d[l, :, nb * 512:(nb + 1) * 512], res[:, par, :], reads=[("adares", par)], writes=["mod_d"])
                    n += 1
            self.barrier()

    def modulate_T(self, hb, sc1, sh, xb, xT, par, rk, pbank, scratch):
        P = self.P
        _tt(P, 'vector', scratch, hb, sc1, ALU.mult, reads=[("hb", par)] + rk, writes=["modtmp"])
        _tt(P, 'gpsimd', xb, scratch, sh, ALU.add, reads=["modtmp"] + rk, writes=["xb"])
        for kc in range(8):
            b = pbank + kc // 4
            _mm(P, self.bank(b, (kc % 4) * 128, (kc % 4 + 1) * 128), xb[:, kc * 128:(kc + 1) * 128], self.ident[:],
                reads=["xb", "ident"], writes=[("ps", b)])
        for hf in range(2):
            _cp(P, 'scalar' if hf == 0 else 'vector', xT[:, hf * 4:(hf + 1) * 4, :],
                self.bank(pbank + hf).rearrange("p (a b) -> p a b", a=4), reads=[("ps", pbank + hf)], writes=["xT"])

    def load_mod(self, layer, mrow, off, dst, key, plus1=False):
        P = self.P
        self.bcast_load(dst, self.dr["mod_d"][layer, mrow, off * D:(off + 1) * D], key)
        if plus1:
            _ts(P, 'gpsimd', dst, dst, 1.0, None, ALU.add, reads=[key], writes=[key])

    def stage_gla_proj(self):
        P, nc = self.P, self.nc
        w_in = self.inp("gla_w_in", [1, D, 3072])
        gfa = self.inp("gla_gate_fwd_a", [1, D, 16])
        gba = self.inp("gla_gate_bwd_a", [1, D, 16])
        gfb = self.inp("gla_gate_fwd_b", [1, 16, 512])
        gbb = self.inp("gla_gate_bwd_b", [1, 16, 512])
        gfbias = self.inp("gla_gate_fwd_bias", [1, 512])
        gbbias = self.inp("gla_gate_bwd_bias", [1, 512])
        R = len(self.batches) * (CT + LT) * 128
        proj_d = self.dram("proj_d", [R, 3072], F32)
        lg_d = self.dram("lg_d", [R, 1024], F32)
        with ExitStack() as st:
            sb = lambda n, s, d: self.sbuf(st, n, s, d)
            wb = sb("w_in_b", [128, 8, 3072], BF16)
            gab = sb("gab", [128, 8, 32], BF16)
            gab32 = sb("gab32", [128, 8, 32], F32)
            gbt = sb("gbt", [32, 1024], BF16)
            gbt32 = sb("gbt32", [32, 1024], F32)
            gbias = sb("gbias", [128, 1024], F32)
            hb = sb("hb", [128, 2, 1024], F32)
            sc1 = sb("sc1", [128, 1024], F32)
            sh = sb("sh", [128, 1024], F32)
            tmp = sb("modtmp", [128, 1024], F32)
            xb = sb("xb", [128, 1024], BF16)
            xT = sb("xT", [128, 8, 128], BF16)
            o32 = sb("o32", [128, 3072], F32)
            g1T = sb("g1T", [32, 128], BF16)
            z = sb("z", [128, 1024], F32)
            wv = w_in[0].rearrange("(kc p) n -> p kc n", p=128)
            self.load_cast(st, [wb[:, kc, :] for kc in range(8)], [wv[:, kc, :] for kc in range(8)], 3072, "w_in_b")
            P.dma('sync', gab32[:, :, 0:16], gfa[0].rearrange("(kc p) n -> p kc n", p=128), writes=["gab32"])
            P.dma('sync', gab32[:, :, 16:32], gba[0].rearrange("(kc p) n -> p kc n", p=128), writes=["gab32"])
            _cp(P, 'vector', gab[:], gab32[:], reads=["gab32"], writes=["gab"])
            _memset(P, 'vector', gbt32[:], 0.0, writes=["gbt32"])
            P.dma('sync', gbt32[0:16, 0:512], gfb[0], reads=["gbt32"], writes=["gbt32a"])
            P.dma('sync', gbt32[16:32, 512:1024], gbb[0], reads=["gbt32"], writes=["gbt32b"])
            _cp(P, 'vector', gbt[:], gbt32[:], reads=["gbt32a", "gbt32b"], writes=["gbt"])
            self.bcast_load(gbias[:, 0:512], gfbias[0], "gbias")
            self.bcast_load(gbias[:, 512:1024], gbbias[0], "gbias2")
            cur = None
            for n, t in enumerate(self.tiles()):
                par = n % 2
                P.dma('sync', hb[:, par, :], self.h_in(t), writes=[("hb", par)])
                mr = self.modrow(t)
                if mr != cur:
                    cur = mr
                    self.load_mod(0, mr, 0, sh[:], "sh")
                    self.load_mod(0, mr, 1, sc1[:], "sc1", plus1=True)
                self.modulate_T(hb[:, par, :], sc1[:], sh[:], xb[:], xT, par, ["sc1", "sh"], 0, tmp[:])
                for nb in range(6):
                    bk = 2 + nb
                    for kc in range(8):
                        _mm(P, self.bank(bk), xT[:, kc, :], wb[:, kc, nb * 512:(nb + 1) * 512], start=(kc == 0), stop=(kc == 7),
                            reads=["xT", "w_in_b"], writes=[("ps", bk)])
                    _cp(P, 'scalar' if nb % 2 == 0 else 'vector', o32[:, nb * 512:(nb + 1) * 512], self.bank(bk),
                        reads=[("ps", bk)], writes=["o32"])
                P.dma('gpsimd', self.rows(proj_d, t), o32[:], reads=["o32"], writes=["proj_d"])
                for kc in range(8):
                    _mm(P, self.psum[0:32, 0:128], gab[:, kc, :], xT[:, kc, :], start=(kc == 0), stop=(kc == 7),
                        reads=["xT", "gab"], writes=[("ps", 0)])
                _cp(P, 'scalar', g1T[:], self.psum[0:32, 0:128], reads=[("ps", 0)], writes=["g1T"])
                for hf in range(2):
                    _mm(P, self.bank(hf), g1T[:], gbt[:, hf * 512:(hf + 1) * 512], reads=["g1T", "gbt"], writes=[("ps", hf)])
                _tt(P, 'vector', z[:], self.psum[:, 0:1024], gbias[:], ALU.add, reads=[("ps", 0), ("ps", 1), "gbias", "gbias2"], writes=["z"])
                _act(P, z[:], z[:], AF.Exp, reads=["z"], writes=["z"], scale=-1.0)
                _act(P, z[:], z[:], AF.Ln, reads=["z"], writes=["z"], bias=1.0)
                P.dma('gpsimd', self.rows(lg_d, t), z[:], reads=["z"], writes=["lg_d"])
            self.barrier()

    def stage_gla_scan(self, dirn):
        P, nc = self.P, self.nc
        R = len(self.batches) * (CT + LT) * 128
        proj_d = self.dr["proj_d"]
        lg_d = self.dr["lg_d"]
        of_d = self.dram("of_d", [R, 1024], F32)
        h1_d = self.dram("h1_d", [R, 1024], F32)
        gmask_d = self.inp("gmask", [128, 4, 128])
        selc_d = self.inp("selc", [128, 2])
        amask_d = self.inp("amask", [128, 2, 4, 128])
        with ExitStack() as st:
            sb = lambda n, s, d: self.sbuf(st, n, s, d)
            gm = sb("gm", [128, 2, 128], F32)
            selc = sb("selc", [128, 2], F32)
            am = sb("am", [128, 4, 128], F32)
            qk = sb("qk", [128, 2, 1024], F32)
            v32 = sb("v32", [128, 2, 1024], F32)
            lgt = sb("lgt", [128, 2, 512], F32)
            vb = sb("vb", [128, 1024], BF16)
            Eq = sb("Eq", [128, 512], F32)
            Ek = sb("Ek", [128, 512], F32)
            Ed = sb("Ed", [128, 512], F32)
            dec = sb("dec", [128, 8], F32)
            qe = sb("qe", [128, 512], BF16)
            ke = sb("ke", [128, 512], BF16)
            kd = sb("kd", [128, 512], BF16)
            qeT = sb("qeT", [128, 4, 128], BF16)
            keT = sb("keT", [128, 4, 128], BF16)
            aTm = sb("aTm", [128, 4, 128], BF16)
            S = sb("S", [128, 4, 256], F32)
            Sb = sb("Sb", [128, 4, 256], BF16)
            P.dma('sync', gm[:], gmask_d[:, 2 * dirn:2 * dirn + 2, :], writes=["gm"])
            P.dma('sync', selc[:], selc_d[:, :], writes=["selc"])
            P.dma('sync', am[:], amask_d[:, dirn], writes=["am"])
            if dirn == 0:
                of32 = sb("of32", [128, 2, 1024], F32)
            else:
                w_out = self.inp("gla_w_out", [1, D, D])
                norm_g = self.inp("gla_norm_g", [1, 256])
                ln_g = self.inp("ln_tm_g", [2, D])
                ln_b = self.inp("ln_tm_b", [2, D])
                wob = sb("wob", [128, 8, 1024], BF16)
                self.load_cast(st, [wob[:, kc, :] for kc in range(8)],
                               [w_out[0].rearrange("(kc p) n -> p kc n", p=128)[:, kc, :] for kc in range(8)], 1024, "wob")
                gn = sb("gn", [128, 4, 256], F32)
                for h in range(4):
                    self.bcast_load(gn[:, h, :], norm_g[0], ("gn", h))
                gnk = [("gn", h) for h in range(4)]
                lng = sb("lng", [128, 1024], F32)
                lnb = sb("lnb", [128, 1024], F32)
                self.bcast_load(lng[:], ln_g[0], "lng")
                self.bcast_load(lnb[:], ln_b[0], "lnb")
                gt = sb("gt", [128, 1024], F32)
                oft = sb("oft", [128, 2, 1024], F32)
                rt = sb("rt", [128, 2, 1024], F32)
                ht = sb("ht", [128, 2, 1024], F32)
                osum = sb("osum", [128, 1024], F32)
                ss = sb("ss", [128, 8], F32)
                sq = sb("sq", [128, 256], F32)
                yb = sb("yb", [128, 1024], BF16)
                yT = sb("yT", [128, 8, 128], BF16)
                zt = sb("zt", [128, 1024], F32)
                hn = sb("hn", [128, 2, 1024], F32)
            n = 0
            curm = None
            for b in self.batches:
                chain = [('c', b, j) for j in range(CT)] + [('l', b, j) for j in range(LT)]
                if dirn == 1:
                    chain = [('c', b, j) for j in reversed(range(CT))] + [('l', b, j) for j in reversed(range(LT))]
                _memset(P, 'vector', S[:], 0.0, writes=[("S", h) for h in range(4)])
                _memset(P, 'gpsimd', Sb[:], 0.0, writes=[("Sb", h) for h in range(4)])
                for t in chain:
                    par = n % 2
                    n += 1
                    pr = self.rows(proj_d, t)
                    P.dma('sync', qk[:, par, :], pr[:, 0:1024], writes=[("qk", par)])
                    P.dma('sync', v32[:, par, :], pr[:, 1024:2048], writes=[("v32", par)])
                    P.dma('sync', lgt[:, par, :], self.rows(lg_d, t)[:, dirn * 512:(dirn + 1) * 512], writes=[("lgt", par)])
                    if dirn == 1:
                        P.dma('sync', oft[:, par, :], self.rows(of_d, t), writes=[("oft", par)])
                        P.dma('sync', rt[:, par, :], pr[:, 2048:3072], writes=[("rt", par)])
                        P.dma('sync', ht[:, par, :], self.h_in(t), writes=[("ht", par)])
                        mr = self.modrow(t)
                        if mr != curm:
                            curm = mr
                            self.load_mod(0, mr, 2, gt[:], "gt")
                    _cp(P, 'gpsimd', vb[:], v32[:, par, :], reads=[("v32", par)], writes=["vb"])
                    _mm(P, self.bank(0), gm[:, 0, :], lgt[:, par, :], reads=["gm", ("lgt", par)], writes=[("ps", 0)])
                    _mm(P, self.bank(1), gm[:, 1, :], lgt[:, par, :], reads=["gm", ("lgt", par)], writes=[("ps", 1)])
                    for h in range(4):
                        _mm(P, self.bank(3, 2 * h, 2 * h + 2), lgt[:, par, h * 128:(h + 1) * 128], selc[:],
                            reads=["selc", ("lgt", par)], writes=[("ps", 3)])
                    _act(P, Eq[:], self.bank(0), AF.Exp, reads=[("ps", 0)], writes=["Eq"])
                    _act(P, Ek[:], self.bank(0), AF.Exp, reads=[("ps", 0)], writes=["Ek"], scale=-1.0)
                    _act(P, Ed[:], self.bank(1), AF.Exp, reads=[("ps", 1)], writes=["Ed"])
                    _act(P, dec[:], self.bank(3, 0, 8), AF.Exp, reads=[("ps", 3)], writes=["dec"])
                    _stt(P, qe[:], qk[:, par, 0:512], 128.0 ** -0.5, Eq[:], ALU.mult, ALU.mult, reads=[("qk", par), "Eq"], writes=["qe"])
                    _tt(P, 'vector', ke[:], qk[:, par, 512:1024], Ek[:], ALU.mult, reads=[("qk", par), "Ek"], writes=["ke"])
                    _tt(P, 'gpsimd', kd[:], qk[:, par, 512:1024], Ed[:], ALU.mult, reads=[("qk", par), "Ed"], writes=["kd"])
                    for h in range(4):
                        _mm(P, self.bank(2, h * 128, (h + 1) * 128), qe[:, h * 128:(h + 1) * 128], self.ident[:],
                            reads=["qe", "ident"], writes=[("ps", 2)])
                    for h in range(4):
                        _mm(P, self.bank(3, h * 128, (h + 1) * 128), ke[:, h * 128:(h + 1) * 128], self.ident[:],
                            reads=["ke", "ident"], writes=[("ps", 3)])
                    _cp(P, 'scalar', qeT[:], self.bank(2).rearrange("p (a b) -> p a b", a=4), reads=[("ps", 2)], writes=["qeT"])
                    _cp(P, 'vector', keT[:], self.bank(3).rearrange("p (a b) -> p a b", a=4), reads=[("ps", 3)], writes=["keT"])
                    for h in range(4):
                        _mm(P, self.bank(0, h * 128, (h + 1) * 128), keT[:, h, :], qeT[:, h, :], reads=["keT", "qeT"], writes=[("ps", 0)])
                    _tt(P, 'vector', aTm[:], self.bank(0).rearrange("p (a b) -> p a b", a=4), am[:], ALU.mult,
                        reads=[("ps", 0), "am"], writes=["aTm"])
                    corder = [0, 1] if dirn == 0 else [1, 0]
                    for h in range(4):
                        ob = 4 + h
                        _mm(P, self.bank(ob, 0, 256), aTm[:, h, :], vb[:, h * 256:(h + 1) * 256], start=True, stop=False,
                            reads=["aTm", "vb"], writes=[("ps", ob)])
                    for ci, c in enumerate(corder):
                        cs = slice(c * 64, (c + 1) * 64)
                        for h in range(4):
                            ob = 4 + h
                            _mm(P, self.psum[cs, ob * 512: ob * 512 + 256], qeT[:, h, cs], Sb[:, h, :], start=False, stop=True,
                                reads=["qeT", ("Sb", h)], writes=[("ps", ob)])
                        for h in range(4):
                            kvb = self.bank(h % 2, 0, 256)
                            kvk = ("ps", h % 2)
                            _mm(P, kvb, kd[cs, h * 128:(h + 1) * 128], vb[cs, h * 256:(h + 1) * 256],
                                reads=["kd", "vb"], writes=[kvk])
                            _stt(P, S[:, h, :], S[:, h, :], dec[:, 2 * h + c:2 * h + c + 1], kvb, ALU.mult, ALU.add,
                                 reads=[kvk, "dec", ("S", h)], writes=[("S", h)])
                            _cp(P, 'scalar' if h % 2 == 0 else 'gpsimd', Sb[:, h, :], S[:, h, :], reads=[("S", h)], writes=[("Sb", h)])
                    okeys = [("ps", 4 + h) for h in range(4)]
                    ov = self.psum[:, 2048:4096].rearrange("p (a b) -> p a b", a=4)[:, :, 0:256]
                    if dirn == 0:
                        _cp(P, 'scalar', of32[:, par, :].rearrange("p (a b) -> p a b", a=4), ov, reads=okeys, writes=[("of32", par)])
                        P.dma('gpsimd', self.rows(of_d, t), of32[:, par, :], reads=[("of32", par)], writes=["of_d"])
                        continue
                    _tt(P, 'vector', osum[:].rearrange("p (a b) -> p a b", a=4), ov, oft[:, par, :].rearrange("p (a b) -> p a b", a=4), ALU.add,
                        reads=okeys + [("oft", par)], writes=["osum"])
                    for h in range(4):
                        _act(P, sq[:], osum[:, h * 256:(h + 1) * 256], AF.Square, reads=["osum"], writes=["sq", "ss"], accum_out=ss[:, h:h + 1])
                    _ts(P, 'vector', ss[:, 4:8], ss[:, 0:4], 1.0 / 256.0, RMS_EPS, ALU.mult, ALU.add, reads=["ss"], writes=["ss2"])
                    _act(P, ss[:, 4:8], ss[:, 4:8], AF.Sqrt, reads=["ss2"], writes=["ss2"])
                    P.op('vector', lambda e: e.reciprocal(out=ss[:, 4:8], in_=ss[:, 4:8]), reads=["ss2"], writes=["ss2"])
                    _act(P, rt[:, par, :], rt[:, par, :], AF.Silu, reads=[("rt", par)], writes=[("rt", par)])
                    for h in range(4):
                        _stt(P, osum[:, h * 256:(h + 1) * 256], osum[:, h * 256:(h + 1) * 256], ss[:, 4 + h:5 + h], rt[:, par, h * 256:(h + 1) * 256],
                             ALU.mult, ALU.mult, reads=["osum", "ss2", ("rt", par)], writes=["osum"])
                    _tt(P, 'gpsimd', yb[:], osum[:], gn[:].rearrange("p a b -> p (a b)"), ALU.mult, reads=["osum"] + gnk, writes=["yb"])
                    for kc in range(8):
                        bk = 2 + kc // 4
                        _mm(P, self.bank(bk, (kc % 4) * 128, (kc % 4 + 1) * 128), yb[:, kc * 128:(kc + 1) * 128], self.ident[:],
                            reads=["yb", "ident"], writes=[("ps", bk)])
                    _cp(P, 'scalar', yT[:, 0:4, :], self.bank(2).rearrange("p (a b) -> p a b", a=4), reads=[("ps", 2)], writes=["yT"])
                    _cp(P, 'vector', yT[:, 4:8, :], self.bank(3).rearrange("p (a b) -> p a b", a=4), reads=[("ps", 3)], writes=["yT"])
                    for hf in range(2):
                        for kc in range(8):
                            _mm(P, self.bank(hf), yT[:, kc, :], wob[:, kc, hf * 512:(hf + 1) * 512], start=(kc == 0), stop=(kc == 7),
                                reads=["yT", "wob"], writes=[("ps", hf)])
                    _tt(P, 'vector', zt[:], self.psum[:, 0:1024], gt[:], ALU.mult, reads=[("ps", 0), ("ps", 1), "gt"], writes=["zt"])
                    _stt(P, zt[:], ht[:, par, :], ALPHA, zt[:], ALU.mult, ALU.add, reads=["zt", ("ht", par)], writes=["zt"])
                    self.layernorm(st, zt[:], hn[:, par, :], lng[:], lnb[:], "zt", ("hn", par), ["lng", "lnb"], "gla")
                    P.dma('gpsimd', self.rows(h1_d, t), hn[:, par, :], reads=[("hn", par)], writes=["h1_d"])
            self.barrier()

    def build(self):
        P = self.P
        NB = self.NB
        self.x_d = self.inp("x", [NB, SEQ, D])
        self.ctx_d = self.inp("ctx", [NB, CTX, D])
        ident_d = self.inp("ident", [128, 128], BF16)
        P.dma('sync', self.ident[:], ident_d[:, :], writes=["ident"])
        self.barrier()
        def run_stage(s):
            if s == 'prep':
                self.stage_prep()
            elif s == 'gla_proj':
                self.stage_gla_proj()
            elif s == 'gla_fwd':
                self.stage_gla_scan(0)
            elif s == 'gla_bwd':
                self.stage_gla_scan(1)
            elif s == 'peer0':
                self.stage_peer(0)
            elif s == 'mla_proj':
                self.stage_mla_proj()
            elif s == 'mla_attn':
                self.stage_mla_attn()
            elif s == 'peer1':
                self.stage_peer(1)
        if 'prep' in self.stages:
            run_stage('prep')
        rest = [s for s in self.stages if s != 'prep']
        if self.batch_outer:
            for b in range(NB):
                self.batches = [b]
                for s in rest:
                    run_stage(s)
        else:
            for s in rest:
                run_stage(s)
        self.barrier()
        P.emit()
        return self.nc


def _peer_prep(self, layer):
    P, nc = self.P, self.nc
    pu = self.inp("peer_u", [2, NEXP, D])
    pv = self.inp("peer_v", [2, NEXP, D])
    UT_d = self.dram("UT_d%d" % layer, [D, NEXP], BF16)
    V_d = self.dram("V_d%d" % layer, [NEXP, D], BF16)
    with ExitStack() as st:
        sb = lambda n, s, d: self.sbuf(st, n, s, d)
        u32 = sb("u32", [128, 2, 4, 1024], F32)
        ub = sb("ub", [128, 2, 4, 1024], BF16)
        uT = sb("uT", [128, 2, 8, 512], BF16)
        v32 = sb("pv32", [128, 2, 4, 1024], F32)
        vbf = sb("pvb", [128, 2, 4, 1024], BF16)
        uview = pu[layer].rearrange("(c p) n -> p c n", p=128)
        vview = pv[layer].rearrange("(c p) n -> p c n", p=128)
        vdview = V_d.rearrange("(c p) n -> p c n", p=128)
        utview = UT_d.rearrange("(kc p) e -> p kc e", p=128)
        for g in range(32):
            par = g % 2
            P.dma('sync', u32[:, par], uview[:, g * 4:(g + 1) * 4, :], writes=[("u32", par)])
            P.dma('sync', v32[:, par], vview[:, g * 4:(g + 1) * 4, :], writes=[("pv32", par)])
            _cp(P, 'gpsimd', ub[:, par], u32[:, par], reads=[("u32", par)], writes=[("ub", par)])
            _cp(P, 'vector', vbf[:, par], v32[:, par], reads=[("pv32", par)], writes=[("pvb", par)])
            P.dma('gpsimd', vdview[:, g * 4:(g + 1) * 4, :], vbf[:, par], reads=[("pvb", par)], writes=["V_d"])
            for c in range(4):
                for kc in range(8):
                    bk = (c % 2) * 2 + kc // 4
                    _mm(P, self.bank(bk, (kc % 4) * 128, (kc % 4 + 1) * 128), ub[:, par, c, kc * 128:(kc + 1) * 128], self.ident[:],
                        reads=[("ub", par), "ident"], writes=[("ps", bk)])
                for hf in range(2):
                    bk = (c % 2) * 2 + hf
                    _cp(P, 'scalar', uT[:, par, hf * 4:(hf + 1) * 4, c * 128:(c + 1) * 128], self.bank(bk).rearrange("p (a b) -> p a b", a=4),
                        reads=[("ps", bk)], writes=[("uT", par)])
            P.dma('gpsimd', utview[:, :, g * 512:(g + 1) * 512], uT[:, par], reads=[("uT", par)], writes=["UT_d"])
        self.barrier()


def _stage_peer(self, layer):
    P, nc = self.P, self.nc
    if ("prep", layer) not in self.done:
        self.done.add(("prep", layer))
        self.peer_prep(layer)
    wq_d = self.inp("peer_w_query", [2, D, 2048])
    ka_d = self.inp("peer_keys_a", [2, 8, 128, 128])
    kb_d = self.inp("peer_keys_b", [2, 8, 128, 128])
    ln_g = self.inp("ln_cm_g", [2, D])
    ln_b = self.inp("ln_cm_b", [2, D])
    R = len(self.batches) * (CT + LT) * 128
    UT_d = self.dr["UT_d%d" % layer]
    V_d = self.dr["V_d%d" % layer]
    utview = UT_d.rearrange("(kc p) e -> p kc e", p=128)
    vdview = V_d.rearrange("(c p) n -> p c n", p=128)
    if layer == 0:
        src = self.dr["h1_d"]
        dst = self.dram("h2_d", [R, D], F32)
        tl = self.tiles(True)
    else:
        src = self.dr["h3_d"]
        dst = self.dram("out", [self.NB * SEQ, D], F32, kind="ExternalOutput")
        tl = self.tiles(False)
    with ExitStack() as st:
        sb = lambda n, s, d: self.sbuf(st, n, s, d)
        wq = sb("wq", [128, 8, 2048], BF16)
        kT = sb("kT", [128, 16, 128], BF16)
        st2 = ExitStack()
        self.load_cast(st2, [wq[:, kc, :] for kc in range(8)],
                       [wq_d[layer].rearrange("(kc p) n -> p kc n", p=128)[:, kc, :] for kc in range(8)], 2048, "wq")
        k32 = self.sbuf(st2, "k32", [128, 16, 128], F32)
        kbf = self.sbuf(st2, "kbf", [128, 16, 128], BF16)
        for h in range(8):
            P.dma('sync', k32[:, 2 * h, :], ka_d[layer, h], writes=[("k32", 2 * h)])
            P.dma('sync', k32[:, 2 * h + 1, :], kb_d[layer, h], writes=[("k32", 2 * h + 1)])
        _cp(P, 'vector', kbf[:], k32[:], reads=[("k32", i) for i in range(16)], writes=["kbf"])
        for blk in range(16):
            bk = blk // 4
            _mm(P, self.bank(bk, (blk % 4) * 128, (blk % 4 + 1) * 128), kbf[:, blk, :], self.ident[:], reads=["kbf", "ident"], writes=[("ps", bk)])
        for bk in range(4):
            _cp(P, 'scalar', kT[:, bk * 4:(bk + 1) * 4, :], self.bank(bk).rearrange("p (a b) -> p a b", a=4), reads=[("ps", bk)], writes=["kT"])
        self.barrier()
        st2.close()
        lng = sb("lng", [128, 1024], F32)
        lnb = sb("lnb", [128, 1024], F32)
        self.bcast_load(lng[:], ln_g[layer], "lng")
        self.bcast_load(lnb[:], ln_b[layer], "lnb")
        sc1 = sb("sc1", [128, 1024], F32)
        sh = sb("sh", [128, 1024], F32)
        gc = sb("gc", [128, 1024], F32)
        tmp = sb("modtmp", [128, 1024], F32)
        hb = sb("hb", [128, 2, 1024], F32)
        xb = sb("xb", [128, 1024], BF16)
        xT1 = sb("xT1", [128, 8, 128], BF16)
        xT = sb("xT", [128, 8, 256], BF16)
        qT = sb("qT", [128, 16, 256], BF16)
        sc = sb("sc", [128, 1, 16, 128], F32)
        v16 = sb("v16", [128, 16, 16], F32)
        mr1 = sb("mr1", [128, 256], F32)
        mr2 = sb("mr2", [128, 256], F32)
        cd = sb("cd", [128, 8, 256], F32)
        c8 = sb("c8", [128, 8, 24], F32)
        stt = sb("stt", [128, 8, 8], F32)
        e16 = sb("e16", [128, 16], F32)
        A = sb("A", [128, 2, 8, 128], F32)
        B = sb("B", [128, 2, 8, 128], F32)
        ethr = sb("ethr", [128, 2, 8], F32)
        Pt = sb("Pt", [128, 1, 8, 512], F32)
        Wh = sb("Wh", [128, 1, 8, 512], BF16)
        utb = sb("utb", [128, 2, 8, 512], BF16)
        vbb = sb("vbb", [128, 3, 4, 1024], BF16)
        Wr = sb("Wr", [128, 5, 512], BF16)
        Ws = sb("Ws", [128, 5, 512], BF16)
        Dg = sb("Dg", [128, 2, 5, 128], BF16)
        nethr = sb("nethr", [128, 2, 8], F32)
        hthb = sb("hthb", [128, 2, 8], BF16)
        cvec = sb("cvec", [128, 2], F32)
        gl = sb("gl", [128, 2, 512], BF16)
        G = sb("G", [128, 2, 512], BF16)
        GT = sb("GT", [128, 2, 4, 128], BF16)
        zt = tmp
        hn = sb("hn", [128, 1, 1024], F32)
        if _os.environ.get("DUMMY_KB"):
            dummy = sb("dummy", [128, int(_os.environ["DUMMY_KB"]) * 256], F32)
        groups = [tl[i:i + 2] for i in range(0, len(tl), 2)]
        curm = None
        nblk = 0
        for grp in groups:
            for tt, t in enumerate(grp):
                P.dma('sync', hb[:, tt, :], self.rows(src, t), writes=[("hb", tt)])
                mr = self.modrow(t)
                if mr != curm:
                    curm = mr
                    self.load_mod(layer, mr, 3, sh[:], "sh")
                    self.load_mod(layer, mr, 4, sc1[:], "sc1", plus1=True)
                    self.load_mod(layer, mr, 5, gc[:], "gc")
                self.modulate_T(hb[:, tt, :], sc1[:], sh[:], xb[:], xT1, tt, ["sc1", "sh"], 0, tmp[:])
                _cp(P, 'gpsimd', xT[:, :, tt * 128:(tt + 1) * 128], xT1[:], reads=["xT"], writes=[("xTg", tt)])
            xk = [("xTg", 0), ("xTg", 1)]
            for half in range(2):
                for b8 in range(8):
                    blk = half * 8 + b8
                    bk = b8 // 2
                    for kc in range(8):
                        _mm(P, self.bank(bk, (b8 % 2) * 256, (b8 % 2) * 256 + 256), wq[:, kc, blk * 128:(blk + 1) * 128], xT[:, kc, :],
                            start=(kc == 0), stop=(kc == 7), reads=["wq"] + xk, writes=[("ps", bk)])
                    _cp(P, 'scalar' if b8 % 2 == 0 else 'vector', qT[:, blk, :], self.bank(bk, (b8 % 2) * 256, (b8 % 2) * 256 + 256),
                        reads=[("ps", bk)], writes=[("qT", blk)])
            qk_ = [("qT", i) for i in range(16)]
            for tt, t in enumerate(grp):
                for blk in range(16):
                    bk = blk // 4
                    _mm(P, self.bank(bk, (blk % 4) * 128, (blk % 4 + 1) * 128), qT[:, blk, tt * 128:(tt + 1) * 128], kT[:, blk, :],
                        reads=qk_ + ["kT"] + [], writes=[("ps", bk)])
                for bk in range(4):
                    _cp(P, 'scalar' if bk % 2 == 0 else 'vector', sc[:, 0, bk * 4:(bk + 1) * 4, :], self.bank(bk).rearrange("p (a b) -> p a b", a=4),
                        reads=[("ps", bk)], writes=["sc"])
                for blk in range(16):
                    P.op('vector', lambda e, blk=blk, tt=tt: e.max(out=v16[:, blk, 0:8], in_=sc[:, 0, blk, :]), reads=["sc"], writes=["v16"])
                    P.op('vector', lambda e, blk=blk, tt=tt: e.match_replace(out=mr1[:, 0:128], in_to_replace=v16[:, blk, 0:8], in_values=sc[:, 0, blk, :], imm_value=-1e30),
                         reads=["sc", "v16"], writes=["mr1"])
                    P.op('vector', lambda e, blk=blk: e.max(out=v16[:, blk, 8:16], in_=mr1[:, 0:128]), reads=["mr1"], writes=["v16"])
                for h in range(8):
                    _tt(P, 'vector', cd[:, h, :].rearrange("p (a b) -> p a b", a=16), v16[:, 2 * h, :].unsqueeze(2).to_broadcast([128, 16, 16]),
                        v16[:, 2 * h + 1, :].unsqueeze(1).to_broadcast([128, 16, 16]), ALU.add, reads=["v16"], writes=["cd"])
                    P.op('vector', lambda e, h=h: e.max(out=c8[:, h, 0:8], in_=cd[:, h, :]), reads=["cd"], writes=["c8"])
                    P.op('vector', lambda e, h=h: e.match_replace(out=mr1[:], in_to_replace=c8[:, h, 0:8], in_values=cd[:, h, :], imm_value=-1e30),
                         reads=["cd", "c8"], writes=["mr1"])
                    P.op('vector', lambda e, h=h: e.max(out=c8[:, h, 8:16], in_=mr1[:]), reads=["mr1"], writes=["c8"])
                    P.op('vector', lambda e, h=h: e.match_replace(out=mr2[:], in_to_replace=c8[:, h, 8:16], in_values=mr1[:], imm_value=-1e30),
                         reads=["mr1", "c8"], writes=["mr2"])
                    P.op('vector', lambda e, h=h: e.max(out=c8[:, h, 16:24], in_=mr2[:]), reads=["mr2"], writes=["c8"])
                v4 = v16[:].rearrange("p (h two) k -> p h two k", two=2)
                _ts(P, 'vector', stt[:, :, 0], c8[:, :, 0], -1.0, None, ALU.mult, reads=["c8"], writes=["stt"])
                for h in range(8):
                    _act(P, e16[:], c8[:, h, 0:16], AF.Exp, reads=["c8", "stt"], writes=["e16", "sttZ"], bias=stt[:, h, 0:1], accum_out=stt[:, h, 1:2])
                _act(P, stt[:, :, 2], stt[:, :, 1], AF.Ln, reads=["sttZ", "stt"], writes=["stt2"])
                _ts(P, 'vector', stt[:, :, 3], v4[:, :, 0, 0], -1.0, None, ALU.mult, reads=["v16", "stt2"], writes=["stt3"])
                _tt(P, 'vector', stt[:, :, 4], v4[:, :, 1, 0], stt[:, :, 2], ALU.add, reads=["v16", "stt2", "stt3"], writes=["stt3"])
                _ts(P, 'vector', stt[:, :, 4], stt[:, :, 4], -1.0, None, ALU.mult, reads=["stt3"], writes=["stt3"])
                _tt(P, 'vector', stt[:, :, 5], c8[:, :, 15], c8[:, :, 16], ALU.add, reads=["c8", "stt3"], writes=["stt3"])
                _stt(P, stt[:, :, 5], stt[:, :, 5], 0.5, stt[:, :, 0], ALU.mult, ALU.add, reads=["stt3", "stt"], writes=["stt3"])
                _tt(P, 'vector', stt[:, :, 5], stt[:, :, 5], stt[:, :, 2], ALU.subtract, reads=["stt3", "stt2"], writes=["stt3"])
                _act(P, ethr[:, tt, :], stt[:, :, 5], AF.Exp, reads=["stt3"], writes=[("ethr", tt)])
                _ts(P, 'vector', nethr[:, tt, :], ethr[:, tt, :], -1.0, None, ALU.mult, reads=[("ethr", tt)], writes=[("nethr", tt)])
                _ts(P, 'vector', hthb[:, tt, :], ethr[:, tt, :], 0.5, None, ALU.mult, reads=[("ethr", tt)], writes=[("hthb", tt)])
                P.op('vector', lambda e, tt=tt: e.tensor_reduce(out=cvec[:, tt:tt + 1], in_=hthb[:, tt, 3:8], axis=AX.X, op=ALU.add),
                     reads=[("hthb", tt)], writes=[("cvec", tt)])
                for hi in range(5):
                    _ts(P, 'vector', Dg[:, tt, hi, :], self.ident[:], hthb[:, tt, 3 + hi:4 + hi], None, ALU.mult,
                        reads=[("hthb", tt), "ident"], writes=[("Dg", tt)])
                for h in range(8):
                    _act(P, A[:, tt, h, :], sc[:, 0, 2 * h, :], AF.Exp, reads=["sc", "stt3"], writes=[("A", tt)], bias=stt[:, h, 3:4])
                    _act(P, B[:, tt, h, :], sc[:, 0, 2 * h + 1, :], AF.Exp, reads=["sc", "stt3"], writes=[("B", tt)], bias=stt[:, h, 4:5])
            units = [(eb, tt) for eb in range(32) for tt in range(len(grp))]
            nu = len(units)
            ACT_H = tuple(range(int(_os.environ.get("ACT_NH", "0"))))

            def load_w(eb):
                P.dma('sync', utb[:, eb % 2], utview[:, :, eb * 512:(eb + 1) * 512], writes=[("utb", eb % 2)])
                P.dma('sync', vbb[:, eb % 3], vdview[:, eb * 4:(eb + 1) * 4, :], writes=[("vbb", eb % 3)])

            def p1_act(u, hs):
                eb, tt = units[u]
                for h in hs:
                    for c in range(4):
                        r = eb * 4 + c
                        _act(P, Pt[:, 0, h, c * 128:(c + 1) * 128], B[:, tt, h, :], AF.Copy, reads=[("A", tt), ("B", tt)], writes=[("Pt", h)],
                             scale=A[:, tt, h, r:r + 1])

            def p1_pool(u):
                eb, tt = units[u]
                for h in range(8):
                    _tt(P, 'vector' if h == 0 else 'gpsimd', Pt[:, 0, h, :].rearrange("p (a b) -> p a b", a=4),
                        A[:, tt, h, eb * 4:(eb + 1) * 4].unsqueeze(2).to_broadcast([128, 4, 128]),
                        B[:, tt, h, :].unsqueeze(1).to_broadcast([128, 4, 128]), ALU.mult,
                        reads=[("A", tt), ("B", tt)], writes=[("Pt", h)])

            def p2_dve(u):
                eb, tt = units[u]
                for h in range(3):
                    _stt(P, Wh[:, 0, h, :], Pt[:, 0, h, :], ethr[:, tt, h:h + 1], Pt[:, 0, h, :], ALU.is_ge, ALU.mult,
                         reads=[("Pt", h), ("ethr", tt)], writes=[("Wh", h)])

            def p2_act(u):
                eb, tt = units[u]
                for hi in range(5):
                    h = 3 + hi
                    _act(P, Wr[:, hi, :], Pt[:, 0, h, :], AF.Relu, reads=[("Pt", h), ("nethr", tt)], writes=[("Wr", hi)], bias=nethr[:, tt, h:h + 1])
                    _act(P, Ws[:, hi, :], Pt[:, 0, h, :], AF.Sign, reads=[("Pt", h), ("nethr", tt)], writes=[("Ws", hi)], bias=nethr[:, tt, h:h + 1])

            def pe_h(u):
                eb, tt = units[u]
                for kc in range(8):
                    _mm(P, self.bank(0), xT[:, kc, tt * 128:(tt + 1) * 128], utb[:, eb % 2, kc, :], start=(kc == 0), stop=(kc == 7),
                        reads=xk + [("utb", eb % 2)], writes=[("ps", 0)])

            def pe_wsum(u):
                eb, tt = units[u]
                wb = 2 + u % 2
                for h in range(3):
                    _mm(P, self.bank(wb), self.ident[:], Wh[:, 0, h, :], start=(h == 0), stop=False,
                        reads=[("Wh", h), "ident"], writes=[("ps", wb)])
                for hi in range(5):
                    _mm(P, self.bank(wb), self.ident[:], Wr[:, hi, :], start=False, stop=False,
                        reads=[("Wr", hi), "ident"], writes=[("ps", wb)])
                for hi in range(5):
                    _mm(P, self.bank(wb), Dg[:, tt, hi, :], Ws[:, hi, :], start=False, stop=(hi == 4),
                        reads=[("Ws", hi), ("Dg", tt)], writes=[("ps", wb)])

            def act_gelu(u):
                _act(P, gl[:, u % 2, :], self.bank(0), AF.Gelu, reads=[("ps", 0)], writes=[("gl", u % 2)])

            def dve_g(u):
                eb, tt = units[u]
                wb = 2 + u % 2
                _stt(P, G[:, u % 2, :], self.bank(wb), cvec[:, tt:tt + 1], gl[:, u % 2, :], ALU.add, ALU.mult,
                     reads=[("gl", u % 2), ("ps", wb), ("cvec", tt)], writes=[("G", u % 2)])

            def pe_t(u):
                for c in range(4):
                    _mm(P, self.bank(1, c * 128, (c + 1) * 128), G[:, u % 2, c * 128:(c + 1) * 128], self.ident[:],
                        reads=[("G", u % 2), "ident"], writes=[("ps", 1)])

            def act_gt(u):
                _cp(P, 'scalar', GT[:, u % 2], self.bank(1).rearrange("p (a b) -> p a b", a=4), reads=[("ps", 1)], writes=[("GT", u % 2)])

            def pe_v(u):
                eb, tt = units[u]
                for c in range(4):
                    for hf in range(2):
                        ob = 4 + tt * 2 + hf
                        _mm(P, self.bank(ob), GT[:, u % 2, c, :], vbb[:, eb % 3, c, hf * 512:(hf + 1) * 512],
                            start=(eb == 0 and c == 0), stop=(eb == 31 and c == 3),
                            reads=[("GT", u % 2), ("vbb", eb % 3)], writes=[("ps", ob)])

            load_w(0)
            p1_pool(0)
            for i in range(nu + 2):
                if i < nu:
                    eb, tt = units[i]
                    if tt == 0 and eb + 1 < 32:
                        load_w(eb + 1)
                if i < nu:
                    p2_act(i)
                if i - 2 >= 0:
                    pe_t(i - 2)
                    act_gt(i - 2)
                if i < nu:
                    pe_h(i)
                    act_gelu(i)
                    p2_dve(i)
                if i + 1 < nu:
                    p1_pool(i + 1)
                if i - 2 >= 0:
                    pe_v(i - 2)
                if i < nu:
                    pe_wsum(i)
                if i - 1 >= 0 and i - 1 < nu:
                    dve_g(i - 1)
            for tt, t in enumerate(grp):
                ob = 4 + tt * 2
                _tt(P, 'vector', zt[:], self.psum[:, ob * 512:(ob + 2) * 512], gc[:], ALU.mult, reads=[("ps", ob), ("ps", ob + 1), "gc"], writes=["modtmp"])
                _stt(P, zt[:], hb[:, tt, :], ALPHA, zt[:], ALU.mult, ALU.add, reads=["modtmp", ("hb", tt)], writes=["modtmp"])
                self.layernorm(st, zt[:], hn[:, 0, :], lng[:], lnb[:], "modtmp", "hn", ["lng", "lnb"], "peer%d" % layer)
                if layer == 0:
                    P.dma('gpsimd', self.rows(dst, t), hn[:, 0, :], reads=["hn"], writes=["dst"])
                else:
                    i = t[1] * LT + t[2]
                    P.dma('gpsimd', dst[i * 128:(i + 1) * 128], hn[:, 0, :], reads=["hn"], writes=["dst"])
        self.barrier()


K.peer_prep = _peer_prep
K.stage_peer = _stage_peer


def _stage_mla_proj(self):
    P, nc = self.P, self.nc
    wdn_d = self.inp("mla_w_down", [1, D, 448])
    qg_d = self.inp("mla_q_norm_g", [1, 256])
    kvg_d = self.inp("mla_kv_norm_g", [1, 128])
    wuq_d = self.inp("mla_w_uq", [1, 256, 1536])
    wukv_d = self.inp("mla_w_ukv", [1, 128, 2048])
    cos_d = self.inp("cos", [SEQ, 32])
    sin_d = self.inp("sin", [SEQ, 32])
    NT = len(self.batches) * (CT + LT)
    src = self.dr["h2_d"]
    Qn_d = self.dram("Qn_d", [NT, 128, 8, 128], BF16)
    Qr_d = self.dram("Qr_d", [NT, 64, 8, 128], BF16)
    Kn_d = self.dram("Kn_d", [NT, 128, 8, 128], BF16)
    Kr_d = self.dram("Kr_d", [NT, 64, 128], BF16)
    Vt_d = self.dram("Vt_d", [NT, 128, 1024], BF16)
    with ExitStack() as st:
        sb = lambda n, s, d: self.sbuf(st, n, s, d)
        wdn = sb("wdn", [128, 8, 448], BF16)
        wuq = sb("wuq", [128, 2, 1536], BF16)
        wukv = sb("wukv", [128, 2048], BF16)
        st2 = ExitStack()
        self.load_cast(st2, [wdn[:, kc, :] for kc in range(8)],
                       [wdn_d[0].rearrange("(kc p) n -> p kc n", p=128)[:, kc, :] for kc in range(8)], 448, "wdn")
        self.load_cast(st2, [wuq[:, kc, :] for kc in range(2)],
                       [wuq_d[0].rearrange("(kc p) n -> p kc n", p=128)[:, kc, :] for kc in range(2)], 1536, "wuq")
        self.load_cast(st2, [wukv[:, :]], [wukv_d[0]], 2048, "wukv")
        self.barrier()
        st2.close()
        qg = sb("qg", [128, 256], F32)
        kvg = sb("kvg", [128, 128], F32)
        self.bcast_load(qg[:], qg_d[0], "qg")
        self.bcast_load(kvg[:], kvg_d[0], "kvg")
        sc1 = sb("sc1", [128, 1024], F32)
        sh = sb("sh", [128, 1024], F32)
        tmp = sb("modtmp", [128, 1024], F32)
        hb = sb("hb", [128, 2, 1024], F32)
        xb = sb("xb", [128, 1024], BF16)
        xT = sb("xT", [128, 8, 128], BF16)
        dn = sb("dn", [128, 448], F32)
        sq = sb("sq", [128, 256], F32)
        ss = sb("ss", [128, 4], F32)
        cqn = sb("cqn", [128, 384], BF16)
        cT = sb("cT", [128, 3, 128], BF16)
        cs = sb("cs", [128, 2, 32], F32)
        q32 = sb("q32", [128, 8, 192], F32)
        r1 = sb("r1", [128, 8, 2, 16], F32)
        r2 = sb("r2", [128, 8, 2, 16], F32)
        qb = sb("qb", [128, 8, 192], BF16)
        kb = sb("kb", [128, 8, 128], BF16)
        krb = sb("krb", [128, 64], BF16)
        vb = sb("vb", [128, 8, 128], BF16)
        qnT = sb("qnT", [128, 8, 128], BF16)
        qrT = sb("qrT", [64, 8, 128], BF16)
        knT = sb("knT", [128, 8, 128], BF16)
        krT = sb("krT", [64, 128], BF16)
        curm = None

        def rope(xv, nh, outv, kx):
            x4 = xv.rearrange("p h (a two f) -> p h a two f", a=2, two=2)
            o4 = outv.rearrange("p h (a two f) -> p h a two f", a=2, two=2)
            cosb = cs[:, 0, :].rearrange("p (a f) -> p a f", a=2).unsqueeze(1).to_broadcast([128, nh, 2, 16])
            sinb = cs[:, 1, :].rearrange("p (a f) -> p a f", a=2).unsqueeze(1).to_broadcast([128, nh, 2, 16])
            x1 = x4[:, :, :, 0, :]
            x2 = x4[:, :, :, 1, :]
            a1 = r1[:, 0:nh]
            a2 = r2[:, 0:nh]
            _tt(P, 'vector', a1, x1, cosb, ALU.mult, reads=kx + ["cs"], writes=["r1"])
            _tt(P, 'vector', a2, x2, sinb, ALU.mult, reads=kx + ["cs"], writes=["r2"])
            _tt(P, 'vector', o4[:, :, :, 0, :], a1, a2, ALU.subtract, reads=["r1", "r2"], writes=["ropeo"])
            _tt(P, 'vector', a1, x1, sinb, ALU.mult, reads=kx + ["cs", "ropeo"], writes=["r1"])
            _tt(P, 'vector', a2, x2, cosb, ALU.mult, reads=kx + ["cs", "ropeo"], writes=["r2"])
            _tt(P, 'vector', o4[:, :, :, 1, :], a1, a2, ALU.add, reads=["r1", "r2"], writes=["ropeo"])

        for n, t in enumerate(self.tiles(True)):
            par = n % 2
            lat = t[0] == 'l'
            ti = self.tid(t)
            P.dma('sync', hb[:, par, :], self.rows(src, t), writes=[("hb", par)])
            if lat:
                P.dma('sync', cs[:, 0, :], cos_d[t[2] * 128:(t[2] + 1) * 128, :], writes=["cs"])
                P.dma('sync', cs[:, 1, :], sin_d[t[2] * 128:(t[2] + 1) * 128, :], writes=["cs2"])
            mr = self.modrow(t)
            if mr != curm:
                curm = mr
                self.load_mod(1, mr, 0, sh[:], "sh")
                self.load_mod(1, mr, 1, sc1[:], "sc1", plus1=True)
            self.modulate_T(hb[:, par, :], sc1[:], sh[:], xb[:], xT, par, ["sc1", "sh"], 0, tmp[:])
            for kc in range(8):
                _mm(P, self.bank(2, 0, 448), xT[:, kc, :], wdn[:, kc, :], start=(kc == 0), stop=(kc == 7), reads=["xT", "wdn"], writes=[("ps", 2)])
            _cp(P, 'scalar', dn[:], self.bank(2, 0, 448), reads=[("ps", 2)], writes=["dn"])
            if DBG == 1:
                continue
            _act(P, sq[:, 0:256], dn[:, 0:256], AF.Square, reads=["dn"], writes=["sq", "ss"], accum_out=ss[:, 0:1])
            _act(P, sq[:, 0:128], dn[:, 256:384], AF.Square, reads=["dn"], writes=["sq", "ss"], accum_out=ss[:, 1:2])
            _ts(P, 'vector', ss[:, 2:3], ss[:, 0:1], 1.0 / 256.0, RMS_EPS, ALU.mult, ALU.add, reads=["ss"], writes=["ss2"])
            _ts(P, 'vector', ss[:, 3:4], ss[:, 1:2], 1.0 / 128.0, RMS_EPS, ALU.mult, ALU.add, reads=["ss"], writes=["ss2"])
            _act(P, ss[:, 2:4], ss[:, 2:4], AF.Sqrt, reads=["ss2"], writes=["ss2"])
            P.op('vector', lambda e: e.reciprocal(out=ss[:, 2:4], in_=ss[:, 2:4]), reads=["ss2"], writes=["ss2"])
            _stt(P, cqn[:, 0:256], dn[:, 0:256], ss[:, 2:3], qg[:], ALU.mult, ALU.mult, reads=["dn", "ss2", "qg"], writes=["cqn"])
            _stt(P, cqn[:, 256:384], dn[:, 256:384], ss[:, 3:4], kvg[:], ALU.mult, ALU.mult, reads=["dn", "ss2", "kvg"], writes=["cqn"])
            for j in range(3):
                _mm(P, self.bank(3, j * 128, (j + 1) * 128), cqn[:, j * 128:(j + 1) * 128], self.ident[:], reads=["cqn", "ident"], writes=[("ps", 3)])
            _cp(P, 'vector', cT[:], self.bank(3, 0, 384).rearrange("p (a b) -> p a b", a=3), reads=[("ps", 3)], writes=["cT"])
            for nb in range(4):
                _mm(P, self.bank(4 + nb), cT[:, 2, :], wukv[:, nb * 512:(nb + 1) * 512], reads=["cT", "wukv"], writes=[("ps", 4 + nb)])
            kvv = self.psum[:, 2048:4096].rearrange("p (h c) -> p h c", h=8)
            _cp(P, 'scalar', kb[:], kvv[:, :, 0:128], reads=[("ps", 4 + i) for i in range(4)], writes=["kb"])
            _cp(P, 'vector', vb[:], kvv[:, :, 128:256], reads=[("ps", 4 + i) for i in range(4)], writes=["vb"])
            P.dma('gpsimd', Vt_d[ti], vb[:].rearrange("p h c -> p (h c)"), reads=["vb"], writes=["Vt_d"])
            if DBG == 2:
                continue
            if lat:
                rope(dn[:, 384:448].unsqueeze(1), 1, krb[:].unsqueeze(1), ["dn", "cs2"])
            else:
                _cp(P, 'vector', krb[:], dn[:, 384:448], reads=["dn"], writes=["ropeo"])
            if DBG == 3:
                continue
            for h in range(8):
                bk = h // 4
                _mm(P, self.bank(bk, (h % 4) * 128, (h % 4 + 1) * 128), kb[:, h, :], self.ident[:], reads=["kb", "ident"], writes=[("ps", bk)])
            _cp(P, 'scalar', knT[:, 0:4, :], self.bank(0).rearrange("p (a b) -> p a b", a=4), reads=[("ps", 0)], writes=["knT"])
            _cp(P, 'vector', knT[:, 4:8, :], self.bank(1).rearrange("p (a b) -> p a b", a=4), reads=[("ps", 1)], writes=["knT"])
            _mm(P, self.psum[0:64, 2 * 512:2 * 512 + 128], krb[:], self.ident[:], reads=["ropeo", "ident"], writes=[("ps", 2)])
            _cp(P, 'scalar', krT[:], self.psum[0:64, 2 * 512:2 * 512 + 128], reads=[("ps", 2)], writes=["krT"])
            P.dma('gpsimd', Kn_d[ti], knT[:], reads=["knT"], writes=["Kn_d"])
            P.dma('gpsimd', Kr_d[ti], krT[:], reads=["krT"], writes=["Kr_d"])
            if not lat or DBG == 4:
                continue
            for nb in range(3):
                for kc in range(2):
                    _mm(P, self.bank(4 + nb), cT[:, kc, :], wuq[:, kc, nb * 512:(nb + 1) * 512], start=(kc == 0), stop=(kc == 1),
                        reads=["cT", "wuq"], writes=[("ps", 4 + nb)])
            _cp(P, 'scalar', q32[:].rearrange("p h c -> p (h c)"), self.psum[:, 2048:2048 + 1536], reads=[("ps", 4 + i) for i in range(3)], writes=["q32"])
            _cp(P, 'gpsimd', qb[:, :, 0:128], q32[:, :, 0:128], reads=["q32"], writes=["qb"])
            rope(q32[:, :, 128:192], 8, qb[:, :, 128:192], ["q32", "cs2", "qb"])
            for h in range(8):
                bk = h // 4
                _mm(P, self.bank(bk, (h % 4) * 128, (h % 4 + 1) * 128), qb[:, h, 0:128], self.ident[:], reads=["qb", "ropeo", "ident"], writes=[("ps", bk)])
            _cp(P, 'scalar', qnT[:, 0:4, :], self.bank(0).rearrange("p (a b) -> p a b", a=4), reads=[("ps", 0)], writes=["qnT"])
            _cp(P, 'vector', qnT[:, 4:8, :], self.bank(1).rearrange("p (a b) -> p a b", a=4), reads=[("ps", 1)], writes=["qnT"])
            for h in range(8):
                bk = 2 + h // 4
                _mm(P, self.psum[0:64, bk * 512 + (h % 4) * 128: bk * 512 + (h % 4 + 1) * 128], qb[:, h, 128:192], self.ident[:],
                    reads=["qb", "ropeo", "ident"], writes=[("ps", bk)])
            _cp(P, 'scalar', qrT[:, 0:4, :], self.psum[0:64, 1024:1536].rearrange("p (a b) -> p a b", a=4), reads=[("ps", 2)], writes=["qrT"])
            _cp(P, 'vector', qrT[:, 4:8, :], self.psum[0:64, 1536:2048].rearrange("p (a b) -> p a b", a=4), reads=[("ps", 3)], writes=["qrT"])
            P.dma('gpsimd', Qn_d[ti], qnT[:], reads=["qnT"], writes=["Qn_d"])
            P.dma('gpsimd', Qr_d[ti], qrT[:], reads=["qrT"], writes=["Qr_d"])
        self.barrier()


def _stage_mla_attn(self):
    P, nc = self.P, self.nc
    wo_d = self.inp("mla_w_out", [1, D, D])
    ln_g = self.inp("ln_tm_g", [2, D])
    ln_b = self.inp("ln_tm_b", [2, D])
    R = len(self.batches) * (CT + LT) * 128
    src = self.dr["h2_d"]
    h3_d = self.dram("h3_d", [R, D], F32)
    Qn_d, Qr_d, Kn_d, Kr_d, Vt_d = [self.dr[k] for k in ("Qn_d", "Qr_d", "Kn_d", "Kr_d", "Vt_d")]
    NK = (CT + LT) * 128
    SCALE = 192.0 ** -0.5
    with ExitStack() as st:
        sb = lambda n, s, d: self.sbuf(st, n, s, d)
        wob = sb("wob", [128, 8, 1024], BF16)
        st2 = ExitStack()
        self.load_cast(st2, [wob[:, kc, :] for kc in range(8)],
                       [wo_d[0].rearrange("(kc p) n -> p kc n", p=128)[:, kc, :] for kc in range(8)], 1024, "wob")
        self.barrier()
        st2.close()
        lng = sb("lng", [128, 1024], F32)
        lnb = sb("lnb", [128, 1024], F32)
        gt = sb("gt", [128, 1024], F32)
        self.bcast_load(lng[:], ln_g[1], "lng")
        self.bcast_load(lnb[:], ln_b[1], "lnb")
        KT = sb("KT", [128, 8, NK], BF16)
        KrT = sb("KrT", [64, NK], BF16)
        V = sb("V", [128, CT + LT, 1024], BF16)
        Qn = sb("Qn", [128, 2, 8, 128], BF16)
        Qr = sb("Qr", [64, 2, 8, 128], BF16)
        ht = sb("ht", [128, 2, 1024], F32)
        Pm = sb("Pm", [128, NK], BF16)
        PT = sb("PT", [128, CT + LT, 128], BF16)
        mx = sb("mx", [128, 4], F32)
        oat = sb("oat", [128, 1024], BF16)
        oT = sb("oT", [128, 8, 128], BF16)
        zt = sb("zt", [128, 1024], F32)
        hn = sb("hn", [128, 2, 1024], F32)
        nq = 0
        nkb = CT + LT
        for b in self.batches:
            base = (b - self.batches[0]) * (CT + LT)
            for j in range(nkb):
                P.dma('sync', KT[:, :, j * 128:(j + 1) * 128], Kn_d[base + j], writes=[("KT", j)])
                P.dma('sync', KrT[:, j * 128:(j + 1) * 128], Kr_d[base + j], writes=[("KrT", j)])
                P.dma('sync', V[:, j, :], Vt_d[base + j], writes=[("V", j)])
            kk = [("KT", j) for j in range(nkb)] + [("KrT", j) for j in range(nkb)]
            vk = [("V", j) for j in range(nkb)]
            self.load_mod(1, b, 2, gt[:], "gt")
            for jq in range(LT):
                t = ('l', b, jq)
                ti = self.tid(t)
                par = nq % 2
                nq += 1
                P.dma('sync', Qn[:, par], Qn_d[ti], writes=[("Qn", par)])
                P.dma('sync', Qr[:, par], Qr_d[ti], writes=[("Qr", par)])
                P.dma('sync', ht[:, par, :], self.rows(src, t), writes=[("ht", par)])
                for h in range(8):
                    for nb in range(5):
                        c0 = nb * 512
                        c1 = min(NK, c0 + 512)
                        _mm(P, self.bank(nb, 0, c1 - c0), Qn[:, par, h, :], KT[:, h, c0:c1], start=True, stop=False,
                            reads=[("Qn", par)] + kk, writes=[("ps", nb)])
                        _mm(P, self.bank(nb, 0, c1 - c0), Qr[:, par, h, :], KrT[:, c0:c1], start=False, stop=True,
                            reads=[("Qr", par)] + kk, writes=[("ps", nb)])
                    sk = [("ps", i) for i in range(5)]
                    P.op('vector', lambda e: e.tensor_reduce(out=mx[:, 0:1], in_=self.psum[:, 0:NK], axis=AX.X, op=ALU.max), reads=sk, writes=["mx"])
                    _ts(P, 'vector', mx[:, 1:2], mx[:, 0:1], -SCALE, None, ALU.mult, reads=["mx"], writes=["mx1"])
                    _act(P, Pm[:], self.psum[:, 0:NK], AF.Exp, reads=sk + ["mx1"], writes=["Pm", "mx2"], bias=mx[:, 1:2], scale=SCALE, accum_out=mx[:, 2:3])
                    P.op('vector', lambda e: e.reciprocal(out=mx[:, 3:4], in_=mx[:, 2:3]), reads=["mx2"], writes=["mx3"])
                    for r0 in range(0, nkb, 8):
                        nr = min(8, nkb - r0)
                        for i in range(nr):
                            bk = 5 + i // 4
                            _mm(P, self.bank(bk, (i % 4) * 128, (i % 4 + 1) * 128), Pm[:, (r0 + i) * 128:(r0 + i + 1) * 128], self.ident[:],
                                reads=["Pm", "ident"], writes=[("ps", bk)])
                        n0 = min(4, nr)
                        _cp(P, 'vector', PT[:, r0:r0 + n0, :], self.bank(5, 0, n0 * 128).rearrange("p (a b) -> p a b", a=n0), reads=[("ps", 5)], writes=["PT"])
                        if nr > 4:
                            _cp(P, 'scalar', PT[:, r0 + 4:r0 + nr, :], self.bank(6, 0, (nr - 4) * 128).rearrange("p (a b) -> p a b", a=nr - 4),
                                reads=[("ps", 6)], writes=["PT"])
                    for kbi in range(nkb):
                        _mm(P, self.bank(7, 0, 128), PT[:, kbi, :], V[:, kbi, h * 128:(h + 1) * 128], start=(kbi == 0), stop=(kbi == nkb - 1),
                            reads=["PT"] + vk, writes=[("ps", 7)])
                    _ts(P, 'vector', oat[:, h * 128:(h + 1) * 128], self.bank(7, 0, 128), mx[:, 3:4], None, ALU.mult, reads=[("ps", 7), "mx3"], writes=["oat"])
                for kc in range(8):
                    bk = 5 + kc // 4
                    _mm(P, self.bank(bk, (kc % 4) * 128, (kc % 4 + 1) * 128), oat[:, kc * 128:(kc + 1) * 128], self.ident[:],
                        reads=["oat", "ident"], writes=[("ps", bk)])
                _cp(P, 'scalar', oT[:, 0:4, :], self.bank(5).rearrange("p (a b) -> p a b", a=4), reads=[("ps", 5)], writes=["oT"])
                _cp(P, 'vector', oT[:, 4:8, :], self.bank(6).rearrange("p (a b) -> p a b", a=4), reads=[("ps", 6)], writes=["oT"])
                for hf in range(2):
                    for kc in range(8):
                        _mm(P, self.bank(5 + hf), oT[:, kc, :], wob[:, kc, hf * 512:(hf + 1) * 512], start=(kc == 0), stop=(kc == 7),
                            reads=["oT", "wob"], writes=[("ps", 5 + hf)])
                _tt(P, 'vector', zt[:], self.psum[:, 5 * 512:7 * 512], gt[:], ALU.mult, reads=[("ps", 5), ("ps", 6), "gt"], writes=["zt"])
                _stt(P, zt[:], ht[:, par, :], ALPHA, zt[:], ALU.mult, ALU.add, reads=["zt", ("ht", par)], writes=["zt"])
                self.layernorm(st, zt[:], hn[:, par, :], lng[:], lnb[:], "zt", ("hn", par), ["lng", "lnb"], "mla")
                P.dma('gpsimd', self.rows(h3_d, t), hn[:, par, :], reads=[("hn", par)], writes=["h3_d"])
        self.barrier()


K.stage_mla_proj = _stage_mla_proj
K.stage_mla_attn = _stage_mla_attn


ALL_STAGES = ['prep', 'gla_proj', 'gla_fwd', 'gla_bwd', 'peer0', 'mla_proj', 'mla_attn', 'peer1']
_CACHE = {}


def kernel(**inputs):
    NB = 4
    k = K(NB, ALL_STAGES)
    nc = k.build()
    consts = host_consts()
    f32 = lambda a: np.ascontiguousarray(np.asarray(a, dtype=np.float32))
    x = f32(inputs['x'])
    ctx = f32(inputs['ctx'])
    c = f32(inputs['c'])
    c_ctx = f32(inputs['c_ctx'])
    shared = {}
    for name in k.dr:
        if name in inputs and name not in ('x', 'ctx'):
            shared[name] = f32(inputs[name])
        elif name in consts:
            shared[name] = consts[name]
    maps = []
    for core in range(NCORES):
        m = dict(shared)
        m['x'] = np.ascontiguousarray(x[core * NB:(core + 1) * NB])
        m['ctx'] = np.ascontiguousarray(ctx[core * NB:(core + 1) * NB])
        cv = np.concatenate([c[core * NB:(core + 1) * NB], c_ctx[None]], 0)
        m['cT'] = np.ascontiguousarray(cv.T.reshape(8, 128, 5).transpose(1, 0, 2))
        maps.append(m)
    res = run_bass_kernel_spmd(nc, maps, core_ids=list(range(NCORES)))
    outs = [np.asarray(res.results[i]['out']).reshape(NB, SEQ, D) for i in range(NCORES)]
    return np.concatenate(outs, axis=0).astype(np.float32)
```

```python
from contextlib import ExitStack
import numpy as np
import ml_dtypes
import concourse.bass as bass
import concourse.mybir as mybir
from concourse.bass_utils import run_bass_kernel_spmd

F32 = mybir.dt.float32
BF16 = mybir.dt.bfloat16
AF = mybir.ActivationFunctionType
ALU = mybir.AluOpType
AX = mybir.AxisListType

EPOCH = 30000
import os as _os
DBG = int(_os.environ.get('MLA_DBG', '0'))
ENGS = ['tensor', 'vector', 'scalar', 'gpsimd', 'sync']


class Prog:
    def __init__(self, n_dma_sems=64):
        self.nc = bass.Bass("TRN2", target_bir_lowering=False)
        self.stack = ExitStack()
        self.ops = {e: [] for e in ENGS}
        self.cnt = {e: 0 for e in ENGS}
        self.nsem = 0
        self.esem = {e: self._newsem() for e in ENGS}
        self.seen = {e: {} for e in ENGS}
        self.lastw = {}
        self.readers = {}
        self.dsem = [self._newsem() for _ in range(n_dma_sems)]
        self.dcnt = [0] * n_dma_sems
        self.n_hw = n_dma_sems - 16
        self.drr = {'hw': 0, 'sw': 0}
        self.n_inst = 0

    def _newsem(self):
        self.nsem += 1
        s = self.stack.enter_context(self.nc.semaphore("s%d" % self.nsem))
        return (self.nsem, s)

    def sb(self, name, shape, dt):
        return self.stack.enter_context(self.nc.sbuf_tensor(name, shape, dt))

    def ps(self, name, shape, dt):
        return self.stack.enter_context(self.nc.psum_tensor(name, shape, dt))

    def _deps(self, eng, reads, writes):
        toks = []
        for k in list(reads) + list(writes):
            t = self.lastw.get(k)
            if t is not None:
                toks.append(t)
        for k in writes:
            toks.extend(self.readers.get(k, ()))
        best = {}
        for (sid, sem, val, teng) in toks:
            if teng == eng and eng == 'tensor':
                continue
            if sid not in best or best[sid][1] < val:
                best[sid] = (sem, val)
        waits = []
        seen = self.seen[eng]
        for sid, (sem, val) in best.items():
            if seen.get(sid, 0) >= val:
                continue
            seen[sid] = val
            waits.append((sem, val))
        return waits

    def _record(self, tok, reads, writes):
        for k in reads:
            lst = self.readers.setdefault(k, [])
            if lst and lst[-1][0] == tok[0]:
                lst[-1] = tok
            else:
                lst.append(tok)
        for k in writes:
            self.lastw[k] = tok
            self.readers[k] = []

    def op(self, eng, fn, reads=(), writes=()):
        psr = [k for k in reads if isinstance(k, tuple) and k[0] == "ps"]
        if psr:
            reads = [k for k in reads if k not in psr]
            writes = list(writes) + psr
        waits = self._deps(eng, reads, writes)
        if self.cnt[eng] >= EPOCH:
            self.esem[eng] = self._newsem()
            self.cnt[eng] = 0
        self.cnt[eng] += 1
        sid, sem = self.esem[eng]
        tok = (sid, sem, self.cnt[eng], eng)
        self.ops[eng].append((waits, fn, sem, 1))
        self._record(tok, reads, writes)
        self.n_inst += 1

    def dma(self, eng, out, in_, reads=(), writes=(), **kw):
        if eng == 'gpsimd':
            i = self.n_hw + self.drr['sw']
            self.drr['sw'] = (self.drr['sw'] + 1) % 16
        else:
            i = self.drr['hw']
            self.drr['hw'] = (self.drr['hw'] + 1) % self.n_hw
        sid, sem = self.dsem[i]
        toks_extra = []
        if self.dcnt[i] > 0:
            toks_extra.append((sid, sem, self.dcnt[i], 'dma'))
        waits = self._deps(eng, reads, writes)
        seen = self.seen[eng]
        for (sid2, sem2, val, _) in toks_extra:
            if seen.get(sid2, 0) < val:
                seen[sid2] = val
                waits.append((sem2, val))
        self.dcnt[i] += 16
        tok = (sid, sem, self.dcnt[i], 'dma')
        self.ops[eng].append((waits, lambda e: e.dma_start(out=out, in_=in_, **kw), sem, 16))
        self._record(tok, reads, writes)
        self.n_inst += 1

    def finish(self, eng, keys):
        waits = self._deps(eng, keys, ())
        self.ops[eng].append((waits, None, None, 0))

    def barrier_keys(self):
        return list(self.lastw.keys())

    def emit(self):
        nc = self.nc
        ops = self.ops
        with nc.Block() as block:
            def replay(e, name):
                for (waits, fn, sem, inc) in ops[name]:
                    for (s, v) in waits:
                        e.wait_ge(s, v)
                    if fn is not None:
                        fn(e).then_inc(sem, inc)

            @block.tensor
            def _(e):
                replay(e, 'tensor')

            @block.vector
            def _(e):
                replay(e, 'vector')

            @block.scalar
            def _(e):
                replay(e, 'scalar')

            @block.gpsimd
            def _(e):
                replay(e, 'gpsimd')

            @block.sync
            def _(e):
                replay(e, 'sync')
        self.stack.close()

    def make_identity(self, ident):
        pass


def _mm(P, out, lhsT, rhs, start=True, stop=True, reads=(), writes=()):
    P.op('tensor', lambda e: e.matmul(out, lhsT=lhsT, rhs=rhs, start=start, stop=stop), reads, writes)


def _act(P, out, in_, func, reads=(), writes=(), **kw):
    P.op('scalar', lambda e: e.activation(out=out, in_=in_, func=func, **kw), reads, writes)


def _tt(P, eng, out, in0, in1, op, reads=(), writes=()):
    P.op(eng, lambda e: e.tensor_tensor(out=out, in0=in0, in1=in1, op=op), reads, writes)


def _ts(P, eng, out, in0, s1, s2, op0, op1=None, reads=(), writes=()):
    if op1 is None:
        P.op(eng, lambda e: e.tensor_scalar(out=out, in0=in0, scalar1=s1, scalar2=None, op0=op0), reads, writes)
    else:
        P.op(eng, lambda e: e.tensor_scalar(out=out, in0=in0, scalar1=s1, scalar2=s2, op0=op0, op1=op1), reads, writes)


def _stt(P, out, in0, scalar, in1, op0, op1, reads=(), writes=()):
    P.op('vector', lambda e: e.scalar_tensor_tensor(out=out, in0=in0, scalar=scalar, in1=in1, op0=op0, op1=op1), reads, writes)


def _cp(P, eng, out, in_, reads=(), writes=()):
    if eng == 'scalar':
        P.op(eng, lambda e: e.activation(out=out, in_=in_, func=AF.Copy), reads, writes)
    else:
        P.op(eng, lambda e: e.tensor_copy(out=out, in_=in_), reads, writes)


def _memset(P, eng, ap, val, reads=(), writes=()):
    P.op(eng, lambda e: e.memset(ap, val), reads, writes)


D = 1024
SEQ = 2048
CTX = 256
DEPTH = 2
ALPHA = (2.0 * DEPTH) ** 0.25
LN_EPS = 1e-5
RMS_EPS = 1e-6
NCORES = 8
LT = SEQ // 128
CT = CTX // 128
NEXP = 16384


def host_consts():
    c = {}
    c['ident'] = np.eye(128, dtype=np.float32).astype(ml_dtypes.bfloat16)
    tp = np.arange(128)[:, None]
    t = np.arange(128)[None, :]
    same = (tp // 64) == (t // 64)
    s = -1.0 / 16.0
    gm = np.zeros((128, 4, 128), np.float32)
    gm[:, 0] = (same & (tp <= t)) * s
    gm[:, 1] = (same & (tp > t)) * s
    gm[:, 2] = (same & (tp >= t)) * s
    gm[:, 3] = (same & (tp < t)) * s
    c['gmask'] = gm
    sel = np.zeros((128, 2), np.float32)
    sel[:64, 0] = s
    sel[64:, 1] = s
    c['selc'] = sel
    j = np.arange(128)[:, None]
    i = np.arange(128)[None, :]
    am = np.zeros((128, 2, 4, 128), np.float32)
    am[:, 0] = ((same & (i >= j)) * 1.0)[:, None, :]
    am[:, 1] = ((same & (j > i)) * 1.0)[:, None, :]
    c['amask'] = am
    row = np.repeat(np.arange(SEQ // 64, dtype=np.float32), 64)
    col = np.tile(np.arange(64, dtype=np.float32), SEQ // 64)
    inv = (10000.0 ** (-np.arange(16, dtype=np.float32) / 16)).astype(np.float32)
    ang = np.stack([row[:, None] * inv, col[:, None] * inv], axis=1)
    c['cos'] = np.cos(ang).astype(np.float32).reshape(SEQ, 32)
    c['sin'] = np.sin(ang).astype(np.float32).reshape(SEQ, 32)
    return c


class K:
    def __init__(self, NB, stages, ext=()):
        self.NB = NB
        self.P = P = Prog()
        self.nc = nc = P.nc
        self.ext = set(ext)
        self.stages = stages
        self.dr = {}
        self.psum = P.ps("psum", [128, 4096], F32)
        self.ident = P.sb("s_ident", [128, 128], BF16)
        self.pool_off = 0
        self.done = set()
        self.batches = list(range(NB))
        self.batch_outer = True

    def sbuf(self, st, n, s, d):
        self.uid = getattr(self, 'uid', 0) + 1
        return st.enter_context(self.nc.sbuf_tensor("s%d_%s" % (self.uid, n), s, d))

    def bank(self, b, c0=0, c1=512):
        return self.psum[:, b * 512 + c0: b * 512 + c1]

    def dram(self, name, shape, dt, kind=None):
        if name in self.dr:
            return self.dr[name]
        if kind is None:
            kind = "Internal"
            if name in self.ext:
                kind = "ExternalOutput"
        t = self.nc.dram_tensor(name, list(shape), dt, kind=kind).ap()
        self.dr[name] = t
        return t

    def inp(self, name, shape, dt=F32):
        return self.dram(name, shape, dt, kind="ExternalInput")

    def tiles(self, with_ctx=True):
        out = []
        for b in self.batches:
            if with_ctx:
                for j in range(CT):
                    out.append(('c', b, j))
            for j in range(LT):
                out.append(('l', b, j))
        return out

    def tid(self, t):
        kind, b, j = t
        return (b - self.batches[0]) * (CT + LT) + (j if kind == 'c' else CT + j)

    def rows(self, ap, t):
        i = self.tid(t)
        return ap[i * 128:(i + 1) * 128]

    def modrow(self, t):
        return 4 if t[0] == 'c' else t[1]

    def h_in(self, t):
        kind, b, j = t
        if kind == 'c':
            return self.ctx_d[b, j * 128:(j + 1) * 128, :]
        return self.x_d[b, j * 128:(j + 1) * 128, :]

    def barrier(self):
        P = self.P
        toks = []
        for e in ENGS:
            sid, sem = P.esem[e]
            if P.cnt[e] > 0:
                toks.append((sid, sem, P.cnt[e]))
        for i, (sid, sem) in enumerate(P.dsem):
            if P.dcnt[i] > 0:
                toks.append((sid, sem, P.dcnt[i]))
        for e in ENGS:
            waits = []
            for (sid, sem, val) in toks:
                if P.seen[e].get(sid, 0) < val:
                    P.seen[e][sid] = val
                    waits.append((sem, val))
            P.ops[e].append((waits, None, None, 0))
        P.lastw = {}
        P.readers = {}

    def load_cast(self, st, dst, src, ncols, tag, eng='gpsimd'):
        P = self.P
        stg = self.sbuf(st, "stg_" + tag, [128, 2, 2048], F32)
        n = 0
        for (d, s, w) in self._chunks(dst, src, ncols):
            par = n % 2
            P.dma('sync', stg[:, par, 0:w], s, writes=[("stg" + tag, par)])
            _cp(P, eng, d, stg[:, par, 0:w], reads=[("stg" + tag, par)], writes=[tag])
            n += 1

    def _chunks(self, dst, src, ncols):
        out = []
        for d, s in zip(dst, src):
            for c0 in range(0, ncols, 2048):
                w = min(2048, ncols - c0)
                out.append((d[:, c0:c0 + w], s[:, c0:c0 + w], w))
        return out

    def bcast_load(self, dst, row_ap, key, eng='sync'):
        self.P.dma(eng, dst, row_ap.partition_broadcast(128), writes=[key])

    def layernorm(self, st, z, out, g_t, b_t, keyz, keyout, reads_extra, tag):
        P = self.P
        tag = tag + "_%d" % id(st)
        if not hasattr(self, '_ln_' + tag):
            setattr(self, '_ln_' + tag, (
                self.sbuf(st, "lnst_" + tag, [128, 2, 6], F32),
                self.sbuf(st, "lnmv_" + tag, [128, 4], F32)))
        stt, mv = getattr(self, '_ln_' + tag)
        ks, km = "lnst_" + tag, "lnmv_" + tag
        for i in range(2):
            P.op('vector', lambda e, i=i: e.bn_stats(out=stt[:, i, :], in_=z[:, i * 512:(i + 1) * 512]), reads=[keyz], writes=[ks])
        P.op('vector', lambda e: e.bn_aggr(out=mv[:, 0:2], in_=stt[:].rearrange("p a b -> p (a b)")), reads=[ks], writes=[km])
        _ts(P, 'vector', mv[:, 2:3], mv[:, 1:2], LN_EPS, None, ALU.add, reads=[km], writes=[km])
        _act(P, mv[:, 2:3], mv[:, 2:3], AF.Sqrt, reads=[km], writes=[km])
        P.op('vector', lambda e: e.reciprocal(out=mv[:, 3:4], in_=mv[:, 2:3]), reads=[km], writes=[km])
        _ts(P, 'vector', z, z, mv[:, 0:1], mv[:, 3:4], ALU.subtract, ALU.mult, reads=[km, keyz], writes=[keyz])
        _tt(P, 'gpsimd', z, z, g_t, ALU.mult, reads=[keyz] + reads_extra, writes=[keyz])
        _tt(P, 'vector', out, z, b_t, ALU.add, reads=[keyz] + reads_extra, writes=[keyout])

    def stage_prep(self):
        P, nc = self.P, self.nc
        cT = self.inp("cT", [128, 8, 5])
        ada_w = self.inp("ada_w", [2, D, 6 * D])
        ada_b = self.inp("ada_b", [2, 6 * D])
        mod_d = self.dram("mod_d", [2, 5, 6 * D], F32)
        with ExitStack() as st:
            sb = lambda n, s, d: self.sbuf(st, n, s, d)
            cs = sb("cs", [128, 8, 5], F32)
            wt = sb("adaw", [128, 2, 8, 512], F32)
            bt = sb("adab", [5, 2, 512], F32)
            res = sb("adares", [5, 2, 512], F32)
            P.dma('sync', cs[:], cT[:, :, :], writes=["cs"])
            _act(P, cs[:], cs[:], AF.Silu, reads=["cs"], writes=["cs"])
            n = 0
            for l in range(2):
                wv = ada_w[l].rearrange("(kc p) n -> p kc n", p=128)
                for nb in range(12):
                    par = n % 2
                    P.dma('sync', wt[:, par], wv[:, :, nb * 512:(nb + 1) * 512], writes=[("adaw", par)])
                    P.dma('gpsimd', bt[:, par, :], ada_b[l, nb * 512:(nb + 1) * 512].partition_broadcast(5), writes=[("adab", par)])
                    for kc in range(8):
                        _mm(P, self.psum[0:5, par * 512:(par + 1) * 512], cs[:, kc, :], wt[:, par, kc, :], start=(kc == 0), stop=(kc == 7),
                            reads=["cs", ("adaw", par)], writes=[("ps", par)])
                    _tt(P, 'vector', res[:, par, :], self.psum[0:5, par * 512:(par + 1) * 512], bt[:, par, :], ALU.add,
                        reads=[("ps", par), ("adab", par)], writes=[("adares", par)])
                    P.dma('gpsimd', mod_d[l, :, nb * 512:(nb + 1) * 512], res[:, par, :], reads=[("adares", par)], writes=["mod_d"])
                    n += 1
            self.barrier()

    def modulate_T(self, hb, sc1, sh, xb, xT, par, rk, pbank, scratch):
        P = self.P
        _tt(P, 'vector', scratch, hb, sc1, ALU.mult, reads=[("hb", par)] + rk, writes=["modtmp"])
        _tt(P, 'gpsimd', xb, scratch, sh, ALU.add, reads=["modtmp"] + rk, writes=["xb"])
        for kc in range(8):
            b = pbank + kc // 4
            _mm(P, self.bank(b, (kc % 4) * 128, (kc % 4 + 1) * 128), xb[:, kc * 128:(kc + 1) * 128], self.ident[:],
                reads=["xb", "ident"], writes=[("ps", b)])
        for hf in range(2):
            _cp(P, 'scalar' if hf == 0 else 'vector', xT[:, hf * 4:(hf + 1) * 4, :],
                self.bank(pbank + hf).rearrange("p (a b) -> p a b", a=4), reads=[("ps", pbank + hf)], writes=["xT"])

    def load_mod(self, layer, mrow, off, dst, key, plus1=False):
        P = self.P
        self.bcast_load(dst, self.dr["mod_d"][layer, mrow, off * D:(off + 1) * D], key)
        if plus1:
            _ts(P, 'gpsimd', dst, dst, 1.0, None, ALU.add, reads=[key], writes=[key])

    def stage_gla_proj(self):
        P, nc = self.P, self.nc
        w_in = self.inp("gla_w_in", [1, D, 3072])
        gfa = self.inp("gla_gate_fwd_a", [1, D, 16])
        gba = self.inp("gla_gate_bwd_a", [1, D, 16])
        gfb = self.inp("gla_gate_fwd_b", [1, 16, 512])
        gbb = self.inp("gla_gate_bwd_b", [1, 16, 512])
        gfbias = self.inp("gla_gate_fwd_bias", [1, 512])
        gbbias = self.inp("gla_gate_bwd_bias", [1, 512])
        R = len(self.batches) * (CT + LT) * 128
        proj_d = self.dram("proj_d", [R, 3072], F32)
        lg_d = self.dram("lg_d", [R, 1024], F32)
        with ExitStack() as st:
            sb = lambda n, s, d: self.sbuf(st, n, s, d)
            wb = sb("w_in_b", [128, 8, 3072], BF16)
            gab = sb("gab", [128, 8, 32], BF16)
            gab32 = sb("gab32", [128, 8, 32], F32)
            gbt = sb("gbt", [32, 1024], BF16)
            gbt32 = sb("gbt32", [32, 1024], F32)
            gbias = sb("gbias", [128, 1024], F32)
            hb = sb("hb", [128, 2, 1024], F32)
            sc1 = sb("sc1", [128, 1024], F32)
            sh = sb("sh", [128, 1024], F32)
            tmp = sb("modtmp", [128, 1024], F32)
            xb = sb("xb", [128, 1024], BF16)
            xT = sb("xT", [128, 8, 128], BF16)
            o32 = sb("o32", [128, 3072], F32)
            g1T = sb("g1T", [32, 128], BF16)
            z = sb("z", [128, 1024], F32)
            wv = w_in[0].rearrange("(kc p) n -> p kc n", p=128)
            self.load_cast(st, [wb[:, kc, :] for kc in range(8)], [wv[:, kc, :] for kc in range(8)], 3072, "w_in_b")
            P.dma('sync', gab32[:, :, 0:16], gfa[0].rearrange("(kc p) n -> p kc n", p=128), writes=["gab32"])
            P.dma('sync', gab32[:, :, 16:32], gba[0].rearrange("(kc p) n -> p kc n", p=128), writes=["gab32"])
            _cp(P, 'vector', gab[:], gab32[:], reads=["gab32"], writes=["gab"])
            _memset(P, 'vector', gbt32[:], 0.0, writes=["gbt32"])
            P.dma('sync', gbt32[0:16, 0:512], gfb[0], reads=["gbt32"], writes=["gbt32a"])
            P.dma('sync', gbt32[16:32, 512:1024], gbb[0], reads=["gbt32"], writes=["gbt32b"])
            _cp(P, 'vector', gbt[:], gbt32[:], reads=["gbt32a", "gbt32b"], writes=["gbt"])
            self.bcast_load(gbias[:, 0:512], gfbias[0], "gbias")
            self.bcast_load(gbias[:, 512:1024], gbbias[0], "gbias2")
            cur = None
            for n, t in enumerate(self.tiles()):
                par = n % 2
                P.dma('sync', hb[:, par, :], self.h_in(t), writes=[("hb", par)])
                mr = self.modrow(t)
                if mr != cur:
                    cur = mr
                    self.load_mod(0, mr, 0, sh[:], "sh")
                    self.load_mod(0, mr, 1, sc1[:], "sc1", plus1=True)
                self.modulate_T(hb[:, par, :], sc1[:], sh[:], xb[:], xT, par, ["sc1", "sh"], 0, tmp[:])
                for nb in range(6):
                    bk = 2 + nb
                    for kc in range(8):
                        _mm(P, self.bank(bk), xT[:, kc, :], wb[:, kc, nb * 512:(nb + 1) * 512], start=(kc == 0), stop=(kc == 7),
                            reads=["xT", "w_in_b"], writes=[("ps", bk)])
                    _cp(P, 'scalar' if nb % 2 == 0 else 'vector', o32[:, nb * 512:(nb + 1) * 512], self.bank(bk),
                        reads=[("ps", bk)], writes=["o32"])
                P.dma('gpsimd', self.rows(proj_d, t), o32[:], reads=["o32"], writes=["proj_d"])
                for kc in range(8):
                    _mm(P, self.psum[0:32, 0:128], gab[:, kc, :], xT[:, kc, :], start=(kc == 0), stop=(kc == 7),
                        reads=["xT", "gab"], writes=[("ps", 0)])
                _cp(P, 'scalar', g1T[:], self.psum[0:32, 0:128], reads=[("ps", 0)], writes=["g1T"])
                for hf in range(2):
                    _mm(P, self.bank(hf), g1T[:], gbt[:, hf * 512:(hf + 1) * 512], reads=["g1T", "gbt"], writes=[("ps", hf)])
                _tt(P, 'vector', z[:], self.psum[:, 0:1024], gbias[:], ALU.add, reads=[("ps", 0), ("ps", 1), "gbias", "gbias2"], writes=["z"])
                _act(P, z[:], z[:], AF.Exp, reads=["z"], writes=["z"], scale=-1.0)
                _act(P, z[:], z[:], AF.Ln, reads=["z"], writes=["z"], bias=1.0)
                P.dma('gpsimd', self.rows(lg_d, t), z[:], reads=["z"], writes=["lg_d"])
            self.barrier()

    def stage_gla_scan(self, dirn):
        P, nc = self.P, self.nc
        R = len(self.batches) * (CT + LT) * 128
        proj_d = self.dr["proj_d"]
        lg_d = self.dr["lg_d"]
        of_d = self.dram("of_d", [R, 1024], F32)
        h1_d = self.dram("h1_d", [R, 1024], F32)
        gmask_d = self.inp("gmask", [128, 4, 128])
        selc_d = self.inp("selc", [128, 2])
        amask_d = self.inp("amask", [128, 2, 4, 128])
        with ExitStack() as st:
            sb = lambda n, s, d: self.sbuf(st, n, s, d)
            gm = sb("gm", [128, 2, 128], F32)
            selc = sb("selc", [128, 2], F32)
            am = sb("am", [128, 4, 128], F32)
            qk = sb("qk", [128, 2, 1024], F32)
            v32 = sb("v32", [128, 2, 1024], F32)
            lgt = sb("lgt", [128, 2, 512], F32)
            vb = sb("vb", [128, 1024], BF16)
            Eq = sb("Eq", [128, 512], F32)
            Ek = sb("Ek", [128, 512], F32)
            Ed = sb("Ed", [128, 512], F32)
            dec = sb("dec", [128, 8], F32)
            qe = sb("qe", [128, 512], BF16)
            ke = sb("ke", [128, 512], BF16)
            kd = sb("kd", [128, 512], BF16)
            qeT = sb("qeT", [128, 4, 128], BF16)
            keT = sb("keT", [128, 4, 128], BF16)
            aTm = sb("aTm", [128, 4, 128], BF16)
            S = sb("S", [128, 4, 256], F32)
            Sb = sb("Sb", [128, 4, 256], BF16)
            P.dma('sync', gm[:], gmask_d[:, 2 * dirn:2 * dirn + 2, :], writes=["gm"])
            P.dma('sync', selc[:], selc_d[:, :], writes=["selc"])
            P.dma('sync', am[:], amask_d[:, dirn], writes=["am"])
            if dirn == 0:
                of32 = sb("of32", [128, 2, 1024], F32)
            else:
                w_out = self.inp("gla_w_out", [1, D, D])
                norm_g = self.inp("gla_norm_g", [1, 256])
                ln_g = self.inp("ln_tm_g", [2, D])
                ln_b = self.inp("ln_tm_b", [2, D])
                wob = sb("wob", [128, 8, 1024], BF16)
                self.load_cast(st, [wob[:, kc, :] for kc in range(8)],
                               [w_out[0].rearrange("(kc p) n -> p kc n", p=128)[:, kc, :] for kc in range(8)], 1024, "wob")
                gn = sb("gn", [128, 4, 256], F32)
                for h in range(4):
                    self.bcast_load(gn[:, h, :], norm_g[0], ("gn", h))
                gnk = [("gn", h) for h in range(4)]
                lng = sb("lng", [128, 1024], F32)
                lnb = sb("lnb", [128, 1024], F32)
                self.bcast_load(lng[:], ln_g[0], "lng")
                self.bcast_load(lnb[:], ln_b[0], "lnb")
                gt = sb("gt", [128, 1024], F32)
                oft = sb("oft", [128, 2, 1024], F32)
                rt = sb("rt", [128, 2, 1024], F32)
                ht = sb("ht", [128, 2, 1024], F32)
                osum = sb("osum", [128, 1024], F32)
                ss = sb("ss", [128, 8], F32)
                sq = sb("sq", [128, 256], F32)
                yb = sb("yb", [128, 1024], BF16)
                yT = sb("yT", [128, 8, 128], BF16)
                zt = sb("zt", [128, 1024], F32)
                hn = sb("hn", [128, 2, 1024], F32)
            n = 0
            curm = None
            for b in self.batches:
                chain = [('c', b, j) for j in range(CT)] + [('l', b, j) for j in range(LT)]
                if dirn == 1:
                    chain = [('c', b, j) for j in reversed(range(CT))] + [('l', b, j) for j in reversed(range(LT))]
                _memset(P, 'vector', S[:], 0.0, writes=[("S", h) for h in range(4)])
                _memset(P, 'gpsimd', Sb[:], 0.0, writes=[("Sb", h) for h in range(4)])
                for t in chain:
                    par = n % 2
                    n += 1
                    pr = self.rows(proj_d, t)
                    P.dma('sync', qk[:, par, :], pr[:, 0:1024], writes=[("qk", par)])
                    P.dma('sync', v32[:, par, :], pr[:, 1024:2048], writes=[("v32", par)])
                    P.dma('sync', lgt[:, par, :], self.rows(lg_d, t)[:, dirn * 512:(dirn + 1) * 512], writes=[("lgt", par)])
                    if dirn == 1:
                        P.dma('sync', oft[:, par, :], self.rows(of_d, t), writes=[("oft", par)])
                        P.dma('sync', rt[:, par, :], pr[:, 2048:3072], writes=[("rt", par)])
                        P.dma('sync', ht[:, par, :], self.h_in(t), writes=[("ht", par)])
                        mr = self.modrow(t)
                        if mr != curm:
                            curm = mr
                            self.load_mod(0, mr, 2, gt[:], "gt")
                    _cp(P, 'gpsimd', vb[:], v32[:, par, :], reads=[("v32", par)], writes=["vb"])
                    _mm(P, self.bank(0), gm[:, 0, :], lgt[:, par, :], reads=["gm", ("lgt", par)], writes=[("ps", 0)])
                    _mm(P, self.bank(1), gm[:, 1, :], lgt[:, par, :], reads=["gm", ("lgt", par)], writes=[("ps", 1)])
                    for h in range(4):
                        _mm(P, self.bank(3, 2 * h, 2 * h + 2), lgt[:, par, h * 128:(h + 1) * 128], selc[:],
                            reads=["selc", ("lgt", par)], writes=[("ps", 3)])
                    _act(P, Eq[:], self.bank(0), AF.Exp, reads=[("ps", 0)], writes=["Eq"])
                    _act(P, Ek[:], self.bank(0), AF.Exp, reads=[("ps", 0)], writes=["Ek"], scale=-1.0)
                    _act(P, Ed[:], self.bank(1), AF.Exp, reads=[("ps", 1)], writes=["Ed"])
                    _act(P, dec[:], self.bank(3, 0, 8), AF.Exp, reads=[("ps", 3)], writes=["dec"])
                    _stt(P, qe[:], qk[:, par, 0:512], 128.0 ** -0.5, Eq[:], ALU.mult, ALU.mult, reads=[("qk", par), "Eq"], writes=["qe"])
                    _tt(P, 'vector', ke[:], qk[:, par, 512:1024], Ek[:], ALU.mult, reads=[("qk", par), "Ek"], writes=["ke"])
                    _tt(P, 'gpsimd', kd[:], qk[:, par, 512:1024], Ed[:], ALU.mult, reads=[("qk", par), "Ed"], writes=["kd"])
                    for h in range(4):
                        _mm(P, self.bank(2, h * 128, (h + 1) * 128), qe[:, h * 128:(h + 1) * 128], self.ident[:],
                            reads=["qe", "ident"], writes=[("ps", 2)])
                    for h in range(4):
                        _mm(P, self.bank(3, h * 128, (h + 1) * 128), ke[:, h * 128:(h + 1) * 128], self.ident[:],
                            reads=["ke", "ident"], writes=[("ps", 3)])
                    _cp(P, 'scalar', qeT[:], self.bank(2).rearrange("p (a b) -> p a b", a=4), reads=[("ps", 2)], writes=["qeT"])
                    _cp(P, 'vector', keT[:], self.bank(3).rearrange("p (a b) -> p a b", a=4), reads=[("ps", 3)], writes=["keT"])
                    for h in range(4):
                        _mm(P, self.bank(0, h * 128, (h + 1) * 128), keT[:, h, :], qeT[:, h, :], reads=["keT", "qeT"], writes=[("ps", 0)])
                    _tt(P, 'vector', aTm[:], self.bank(0).rearrange("p (a b) -> p a b", a=4), am[:], ALU.mult,
                        reads=[("ps", 0), "am"], writes=["aTm"])
                    corder = [0, 1] if dirn == 0 else [1, 0]
                    for h in range(4):
                        ob = 4 + h
                        _mm(P, self.bank(ob, 0, 256), aTm[:, h, :], vb[:, h * 256:(h + 1) * 256], start=True, stop=False,
                            reads=["aTm", "vb"], writes=[("ps", ob)])
                    for ci, c in enumerate(corder):
                        cs = slice(c * 64, (c + 1) * 64)
                        for h in range(4):
                            ob = 4 + h
                            _mm(P, self.psum[cs, ob * 512: ob * 512 + 256], qeT[:, h, cs], Sb[:, h, :], start=False, stop=True,
                                reads=["qeT", ("Sb", h)], writes=[("ps", ob)])
                        for h in range(4):
                            kvb = self.bank(h % 2, 0, 256)
                            kvk = ("ps", h % 2)
                            _mm(P, kvb, kd[cs, h * 128:(h + 1) * 128], vb[cs, h * 256:(h + 1) * 256],
                                reads=["kd", "vb"], writes=[kvk])
                            _stt(P, S[:, h, :], S[:, h, :], dec[:, 2 * h + c:2 * h + c + 1], kvb, ALU.mult, ALU.add,
                                 reads=[kvk, "dec", ("S", h)], writes=[("S", h)])
                            _cp(P, 'scalar' if h % 2 == 0 else 'gpsimd', Sb[:, h, :], S[:, h, :], reads=[("S", h)], writes=[("Sb", h)])
                    okeys = [("ps", 4 + h) for h in range(4)]
                    ov = self.psum[:, 2048:4096].rearrange("p (a b) -> p a b", a=4)[:, :, 0:256]
                    if dirn == 0:
                        _cp(P, 'scalar', of32[:, par, :].rearrange("p (a b) -> p a b", a=4), ov, reads=okeys, writes=[("of32", par)])
                        P.dma('gpsimd', self.rows(of_d, t), of32[:, par, :], reads=[("of32", par)], writes=["of_d"])
                        continue
                    _tt(P, 'vector', osum[:].rearrange("p (a b) -> p a b", a=4), ov, oft[:, par, :].rearrange("p (a b) -> p a b", a=4), ALU.add,
                        reads=okeys + [("oft", par)], writes=["osum"])
                    for h in range(4):
                        _act(P, sq[:], osum[:, h * 256:(h + 1) * 256], AF.Square, reads=["osum"], writes=["sq", "ss"], accum_out=ss[:, h:h + 1])
                    _ts(P, 'vector', ss[:, 4:8], ss[:, 0:4], 1.0 / 256.0, RMS_EPS, ALU.mult, ALU.add, reads=["ss"], writes=["ss2"])
                    _act(P, ss[:, 4:8], ss[:, 4:8], AF.Sqrt, reads=["ss2"], writes=["ss2"])
                    P.op('vector', lambda e: e.reciprocal(out=ss[:, 4:8], in_=ss[:, 4:8]), reads=["ss2"], writes=["ss2"])
                    _act(P, rt[:, par, :], rt[:, par, :], AF.Silu, reads=[("rt", par)], writes=[("rt", par)])
                    for h in range(4):
                        _stt(P, osum[:, h * 256:(h + 1) * 256], osum[:, h * 256:(h + 1) * 256], ss[:, 4 + h:5 + h], rt[:, par, h * 256:(h + 1) * 256],
                             ALU.mult, ALU.mult, reads=["osum", "ss2", ("rt", par)], writes=["osum"])
                    _tt(P, 'gpsimd', yb[:], osum[:], gn[:].rearrange("p a b -> p (a b)"), ALU.mult, reads=["osum"] + gnk, writes=["yb"])
                    for kc in range(8):
                        bk = 2 + kc // 4
                        _mm(P, self.bank(bk, (kc % 4) * 128, (kc % 4 + 1) * 128), yb[:, kc * 128:(kc + 1) * 128], self.ident[:],
                            reads=["yb", "ident"], writes=[("ps", bk)])
                    _cp(P, 'scalar', yT[:, 0:4, :], self.bank(2).rearrange("p (a b) -> p a b", a=4), reads=[("ps", 2)], writes=["yT"])
                    _cp(P, 'vector', yT[:, 4:8, :], self.bank(3).rearrange("p (a b) -> p a b", a=4), reads=[("ps", 3)], writes=["yT"])
                    for hf in range(2):
                        for kc in range(8):
                            _mm(P, self.bank(hf), yT[:, kc, :], wob[:, kc, hf * 512:(hf + 1) * 512], start=(kc == 0), stop=(kc == 7),
                                reads=["yT", "wob"], writes=[("ps", hf)])
                    _tt(P, 'vector', zt[:], self.psum[:, 0:1024], gt[:], ALU.mult, reads=[("ps", 0), ("ps", 1), "gt"], writes=["zt"])
                    _stt(P, zt[:], ht[:, par, :], ALPHA, zt[:], ALU.mult, ALU.add, reads=["zt", ("ht", par)], writes=["zt"])
                    self.layernorm(st, zt[:], hn[:, par, :], lng[:], lnb[:], "zt", ("hn", par), ["lng", "lnb"], "gla")
                    P.dma('gpsimd', self.rows(h1_d, t), hn[:, par, :], reads=[("hn", par)], writes=["h1_d"])
            self.barrier()

    def build(self):
        P = self.P
        NB = self.NB
        self.x_d = self.inp("x", [NB, SEQ, D])
        self.ctx_d = self.inp("ctx", [NB, CTX, D])
        ident_d = self.inp("ident", [128, 128], BF16)
        P.dma('sync', self.ident[:], ident_d[:, :], writes=["ident"])
        self.barrier()
        def run_stage(s):
            if s == 'prep':
                self.stage_prep()
            elif s == 'gla_proj':
                self.stage_gla_proj()
            elif s == 'gla_fwd':
                self.stage_gla_scan(0)
            elif s == 'gla_bwd':
                self.stage_gla_scan(1)
            elif s == 'peer0':
                self.stage_peer(0)
            elif s == 'mla_proj':
                self.stage_mla_proj()
            elif s == 'mla_attn':
                self.stage_mla_attn()
            elif s == 'peer1':
                self.stage_peer(1)
        if 'prep' in self.stages:
            run_stage('prep')
        rest = [s for s in self.stages if s != 'prep']
        if self.batch_outer:
            for b in range(NB):
                self.batches = [b]
                for s in rest:
                    run_stage(s)
        else:
            for s in rest:
                run_stage(s)
        self.barrier()
        P.emit()
        return self.nc


def _peer_prep(self, layer):
    P, nc = self.P, self.nc
    pu = self.inp("peer_u", [2, NEXP, D])
    pv = self.inp("peer_v", [2, NEXP, D])
    UT_d = self.dram("UT_d%d" % layer, [D, NEXP], BF16)
    V_d = self.dram("V_d%d" % layer, [NEXP, D], BF16)
    with ExitStack() as st:
        sb = lambda n, s, d: self.sbuf(st, n, s, d)
        u32 = sb("u32", [128, 2, 4, 1024], F32)
        ub = sb("ub", [128, 2, 4, 1024], BF16)
        uT = sb("uT", [128, 2, 8, 512], BF16)
        v32 = sb("pv32", [128, 2, 4, 1024], F32)
        vbf = sb("pvb", [128, 2, 4, 1024], BF16)
        uview = pu[layer].rearrange("(c p) n -> p c n", p=128)
        vview = pv[layer].rearrange("(c p) n -> p c n", p=128)
        vdview = V_d.rearrange("(c p) n -> p c n", p=128)
        utview = UT_d.rearrange("(kc p) e -> p kc e", p=128)
        for g in range(32):
            par = g % 2
            P.dma('sync', u32[:, par], uview[:, g * 4:(g + 1) * 4, :], writes=[("u32", par)])
            P.dma('sync', v32[:, par], vview[:, g * 4:(g + 1) * 4, :], writes=[("pv32", par)])
            _cp(P, 'gpsimd', ub[:, par], u32[:, par], reads=[("u32", par)], writes=[("ub", par)])
            _cp(P, 'vector', vbf[:, par], v32[:, par], reads=[("pv32", par)], writes=[("pvb", par)])
            P.dma('gpsimd', vdview[:, g * 4:(g + 1) * 4, :], vbf[:, par], reads=[("pvb", par)], writes=["V_d"])
            for c in range(4):
                for kc in range(8):
                    bk = (c % 2) * 2 + kc // 4
                    _mm(P, self.bank(bk, (kc % 4) * 128, (kc % 4 + 1) * 128), ub[:, par, c, kc * 128:(kc + 1) * 128], self.ident[:],
                        reads=[("ub", par), "ident"], writes=[("ps", bk)])
                for hf in range(2):
                    bk = (c % 2) * 2 + hf
                    _cp(P, 'scalar', uT[:, par, hf * 4:(hf + 1) * 4, c * 128:(c + 1) * 128], self.bank(bk).rearrange("p (a b) -> p a b", a=4),
                        reads=[("ps", bk)], writes=[("uT", par)])
            P.dma('gpsimd', utview[:, :, g * 512:(g + 1) * 512], uT[:, par], reads=[("uT", par)], writes=["UT_d"])
        self.barrier()


def _stage_peer(self, layer):
    P, nc = self.P, self.nc
    if ("prep", layer) not in self.done:
        self.done.add(("prep", layer))
        self.peer_prep(layer)
    wq_d = self.inp("peer_w_query", [2, D, 2048])
    ka_d = self.inp("peer_keys_a", [2, 8, 128, 128])
    kb_d = self.inp("peer_keys_b", [2, 8, 128, 128])
    ln_g = self.inp("ln_cm_g", [2, D])
    ln_b = self.inp("ln_cm_b", [2, D])
    R = len(self.batches) * (CT + LT) * 128
    UT_d = self.dr["UT_d%d" % layer]
    V_d = self.dr["V_d%d" % layer]
    utview = UT_d.rearrange("(kc p) e -> p kc e", p=128)
    vdview = V_d.rearrange("(c p) n -> p c n", p=128)
    if layer == 0:
        src = self.dr["h1_d"]
        dst = self.dram("h2_d", [R, D], F32)
        tl = self.tiles(True)
    else:
        src = self.dr["h3_d"]
        dst = self.dram("out", [self.NB * SEQ, D], F32, kind="ExternalOutput")
        tl = self.tiles(False)
    with ExitStack() as st:
        sb = lambda n, s, d: self.sbuf(st, n, s, d)
        wq = sb("wq", [128, 8, 2048], BF16)
        kT = sb("kT", [128, 16, 128], BF16)
        st2 = ExitStack()
        self.load_cast(st2, [wq[:, kc, :] for kc in range(8)],
                       [wq_d[layer].rearrange("(kc p) n -> p kc n", p=128)[:, kc, :] for kc in range(8)], 2048, "wq")
        k32 = self.sbuf(st2, "k32", [128, 16, 128], F32)
        kbf = self.sbuf(st2, "kbf", [128, 16, 128], BF16)
        for h in range(8):
            P.dma('sync', k32[:, 2 * h, :], ka_d[layer, h], writes=[("k32", 2 * h)])
            P.dma('sync', k32[:, 2 * h + 1, :], kb_d[layer, h], writes=[("k32", 2 * h + 1)])
        _cp(P, 'vector', kbf[:], k32[:], reads=[("k32", i) for i in range(16)], writes=["kbf"])
        for blk in range(16):
            bk = blk // 4
            _mm(P, self.bank(bk, (blk % 4) * 128, (blk % 4 + 1) * 128), kbf[:, blk, :], self.ident[:], reads=["kbf", "ident"], writes=[("ps", bk)])
        for bk in range(4):
            _cp(P, 'scalar', kT[:, bk * 4:(bk + 1) * 4, :], self.bank(bk).rearrange("p (a b) -> p a b", a=4), reads=[("ps", bk)], writes=["kT"])
        self.barrier()
        st2.close()
        lng = sb("lng", [128, 1024], F32)
        lnb = sb("lnb", [128, 1024], F32)
        self.bcast_load(lng[:], ln_g[layer], "lng")
        self.bcast_load(lnb[:], ln_b[layer], "lnb")
        sc1 = sb("sc1", [128, 1024], F32)
        sh = sb("sh", [128, 1024], F32)
        gc = sb("gc", [128, 1024], F32)
        tmp = sb("modtmp", [128, 1024], F32)
        hb = sb("hb", [128, 2, 1024], F32)
        xb = sb("xb", [128, 1024], BF16)
        xT1 = sb("xT1", [128, 8, 128], BF16)
        xT = sb("xT", [128, 8, 256], BF16)
        qT = sb("qT", [128, 16, 256], BF16)
        sc = sb("sc", [128, 1, 16, 128], F32)
        v16 = sb("v16", [128, 16, 16], F32)
        mr1 = sb("mr1", [128, 256], F32)
        mr2 = sb("mr2", [128, 256], F32)
        cd = sb("cd", [128, 8, 256], F32)
        c8 = sb("c8", [128, 8, 24], F32)
        stt = sb("stt", [128, 8, 8], F32)
        e16 = sb("e16", [128, 16], F32)
        A = sb("A", [128, 2, 8, 128], F32)
        B = sb("B", [128, 2, 8, 128], F32)
        ethr = sb("ethr", [128, 2, 8], F32)
        Pt = sb("Pt", [128, 1, 8, 512], F32)
        Wh = sb("Wh", [128, 1, 8, 512], BF16)
        utb = sb("utb", [128, 2, 8, 512], BF16)
        vbb = sb("vbb", [128, 3, 4, 1024], BF16)
        Wr = sb("Wr", [128, 3, 512], BF16)
        Ws = sb("Ws", [128, 3, 512], BF16)
        Dg = sb("Dg", [128, 2, 3, 128], BF16)
        nethr = sb("nethr", [128, 2, 8], F32)
        hthb = sb("hthb", [128, 2, 8], BF16)
        cvec = sb("cvec", [128, 2], F32)
        gl = sb("gl", [128, 2, 512], BF16)
        G = sb("G", [128, 2, 512], BF16)
        GT = sb("GT", [128, 2, 4, 128], BF16)
        zt = tmp
        hn = sb("hn", [128, 1, 1024], F32)
        if _os.environ.get("DUMMY_KB"):
            dummy = sb("dummy", [128, int(_os.environ["DUMMY_KB"]) * 256], F32)
        groups = [tl[i:i + 2] for i in range(0, len(tl), 2)]
        curm = None
        nblk = 0
        for grp in groups:
            for tt, t in enumerate(grp):
                P.dma('sync', hb[:, tt, :], self.rows(src, t), writes=[("hb", tt)])
                mr = self.modrow(t)
                if mr != curm:
                    curm = mr
                    self.load_mod(layer, mr, 3, sh[:], "sh")
                    self.load_mod(layer, mr, 4, sc1[:], "sc1", plus1=True)
                    self.load_mod(layer, mr, 5, gc[:], "gc")
                self.modulate_T(hb[:, tt, :], sc1[:], sh[:], xb[:], xT1, tt, ["sc1", "sh"], 0, tmp[:])
                _cp(P, 'gpsimd', xT[:, :, tt * 128:(tt + 1) * 128], xT1[:], reads=["xT"], writes=[("xTg", tt)])
            xk = [("xTg", 0), ("xTg", 1)]
            for half in range(2):
                for b8 in range(8):
                    blk = half * 8 + b8
                    bk = b8 // 2
                    for kc in range(8):
                        _mm(P, self.bank(bk, (b8 % 2) * 256, (b8 % 2) * 256 + 256), wq[:, kc, blk * 128:(blk + 1) * 128], xT[:, kc, :],
                            start=(kc == 0), stop=(kc == 7), reads=["wq"] + xk, writes=[("ps", bk)])
                    _cp(P, 'scalar' if b8 % 2 == 0 else 'vector', qT[:, blk, :], self.bank(bk, (b8 % 2) * 256, (b8 % 2) * 256 + 256),
                        reads=[("ps", bk)], writes=[("qT", blk)])
            qk_ = [("qT", i) for i in range(16)]
            for tt, t in enumerate(grp):
                for blk in range(16):
                    bk = blk // 4
                    _mm(P, self.bank(bk, (blk % 4) * 128, (blk % 4 + 1) * 128), qT[:, blk, tt * 128:(tt + 1) * 128], kT[:, blk, :],
                        reads=qk_ + ["kT"] + [], writes=[("ps", bk)])
                for bk in range(4):
                    _cp(P, 'scalar' if bk % 2 == 0 else 'vector', sc[:, 0, bk * 4:(bk + 1) * 4, :], self.bank(bk).rearrange("p (a b) -> p a b", a=4),
                        reads=[("ps", bk)], writes=["sc"])
                for blk in range(16):
                    P.op('vector', lambda e, blk=blk, tt=tt: e.max(out=v16[:, blk, 0:8], in_=sc[:, 0, blk, :]), reads=["sc"], writes=["v16"])
                    P.op('vector', lambda e, blk=blk, tt=tt: e.match_replace(out=mr1[:, 0:128], in_to_replace=v16[:, blk, 0:8], in_values=sc[:, 0, blk, :], imm_value=-1e30),
                         reads=["sc", "v16"], writes=["mr1"])
                    P.op('vector', lambda e, blk=blk: e.max(out=v16[:, blk, 8:16], in_=mr1[:, 0:128]), reads=["mr1"], writes=["v16"])
                for h in range(8):
                    _tt(P, 'vector', cd[:, h, :].rearrange("p (a b) -> p a b", a=16), v16[:, 2 * h, :].unsqueeze(2).to_broadcast([128, 16, 16]),
                        v16[:, 2 * h + 1, :].unsqueeze(1).to_broadcast([128, 16, 16]), ALU.add, reads=["v16"], writes=["cd"])
                    P.op('vector', lambda e, h=h: e.max(out=c8[:, h, 0:8], in_=cd[:, h, :]), reads=["cd"], writes=["c8"])
                    P.op('vector', lambda e, h=h: e.match_replace(out=mr1[:], in_to_replace=c8[:, h, 0:8], in_values=cd[:, h, :], imm_value=-1e30),
                         reads=["cd", "c8"], writes=["mr1"])
                    P.op('vector', lambda e, h=h: e.max(out=c8[:, h, 8:16], in_=mr1[:]), reads=["mr1"], writes=["c8"])
                    P.op('vector', lambda e, h=h: e.match_replace(out=mr2[:], in_to_replace=c8[:, h, 8:16], in_values=mr1[:], imm_value=-1e30),
                         reads=["mr1", "c8"], writes=["mr2"])
                    P.op('vector', lambda e, h=h: e.max(out=c8[:, h, 16:24], in_=mr2[:]), reads=["mr2"], writes=["c8"])
                v4 = v16[:].rearrange("p (h two) k -> p h two k", two=2)
                _ts(P, 'vector', stt[:, :, 0], c8[:, :, 0], -1.0, None, ALU.mult, reads=["c8"], writes=["stt"])
                for h in range(8):
                    _act(P, e16[:], c8[:, h, 0:16], AF.Exp, reads=["c8", "stt"], writes=["e16", "sttZ"], bias=stt[:, h, 0:1], accum_out=stt[:, h, 1:2])
                _act(P, stt[:, :, 2], stt[:, :, 1], AF.Ln, reads=["sttZ", "stt"], writes=["stt2"])
                _ts(P, 'vector', stt[:, :, 3], v4[:, :, 0, 0], -1.0, None, ALU.mult, reads=["v16", "stt2"], writes=["stt3"])
                _tt(P, 'vector', stt[:, :, 4], v4[:, :, 1, 0], stt[:, :, 2], ALU.add, reads=["v16", "stt2", "stt3"], writes=["stt3"])
                _ts(P, 'vector', stt[:, :, 4], stt[:, :, 4], -1.0, None, ALU.mult, reads=["stt3"], writes=["stt3"])
                _tt(P, 'vector', stt[:, :, 5], c8[:, :, 15], c8[:, :, 16], ALU.add, reads=["c8", "stt3"], writes=["stt3"])
                _stt(P, stt[:, :, 5], stt[:, :, 5], 0.5, stt[:, :, 0], ALU.mult, ALU.add, reads=["stt3", "stt"], writes=["stt3"])
                _tt(P, 'vector', stt[:, :, 5], stt[:, :, 5], stt[:, :, 2], ALU.subtract, reads=["stt3", "stt2"], writes=["stt3"])
                _act(P, ethr[:, tt, :], stt[:, :, 5], AF.Exp, reads=["stt3"], writes=[("ethr", tt)])
                _ts(P, 'vector', nethr[:, tt, :], ethr[:, tt, :], -1.0, None, ALU.mult, reads=[("ethr", tt)], writes=[("nethr", tt)])
                _ts(P, 'vector', hthb[:, tt, :], ethr[:, tt, :], 0.5, None, ALU.mult, reads=[("ethr", tt)], writes=[("hthb", tt)])
                P.op('vector', lambda e, tt=tt: e.tensor_reduce(out=cvec[:, tt:tt + 1], in_=hthb[:, tt, 5:8], axis=AX.X, op=ALU.add),
                     reads=[("hthb", tt)], writes=[("cvec", tt)])
                for hi in range(3):
                    _ts(P, 'vector', Dg[:, tt, hi, :], self.ident[:], hthb[:, tt, 5 + hi:6 + hi], None, ALU.mult,
                        reads=[("hthb", tt), "ident"], writes=[("Dg", tt)])
                for h in range(8):
                    _act(P, A[:, tt, h, :], sc[:, 0, 2 * h, :], AF.Exp, reads=["sc", "stt3"], writes=[("A", tt)], bias=stt[:, h, 3:4])
                    _act(P, B[:, tt, h, :], sc[:, 0, 2 * h + 1, :], AF.Exp, reads=["sc", "stt3"], writes=[("B", tt)], bias=stt[:, h, 4:5])
            units = [(eb, tt) for eb in range(32) for tt in range(len(grp))]
            nu = len(units)
            ACT_H = tuple(range(int(_os.environ.get("ACT_NH", "0"))))

            def load_w(eb):
                P.dma('sync', utb[:, eb % 2], utview[:, :, eb * 512:(eb + 1) * 512], writes=[("utb", eb % 2)])
                P.dma('sync', vbb[:, eb % 3], vdview[:, eb * 4:(eb + 1) * 4, :], writes=[("vbb", eb % 3)])

            def p1_act(u, hs):
                eb, tt = units[u]
                for h in hs:
                    for c in range(4):
                        r = eb * 4 + c
                        _act(P, Pt[:, 0, h, c * 128:(c + 1) * 128], B[:, tt, h, :], AF.Copy, reads=[("A", tt), ("B", tt)], writes=[("Pt", h)],
                             scale=A[:, tt, h, r:r + 1])

            def p1_pool(u):
                eb, tt = units[u]
                for h in range(8):
                    _tt(P, 'vector' if h == 0 else 'gpsimd', Pt[:, 0, h, :].rearrange("p (a b) -> p a b", a=4),
                        A[:, tt, h, eb * 4:(eb + 1) * 4].unsqueeze(2).to_broadcast([128, 4, 128]),
                        B[:, tt, h, :].unsqueeze(1).to_broadcast([128, 4, 128]), ALU.mult,
                        reads=[("A", tt), ("B", tt)], writes=[("Pt", h)])

            def p2_dve(u):
                eb, tt = units[u]
                for h in range(5):
                    _stt(P, Wh[:, 0, h, :], Pt[:, 0, h, :], ethr[:, tt, h:h + 1], Pt[:, 0, h, :], ALU.is_ge, ALU.mult,
                         reads=[("Pt", h), ("ethr", tt)], writes=[("Wh", h)])

            def p2_act(u):
                eb, tt = units[u]
                for hi in range(3):
                    h = 5 + hi
                    _act(P, Wr[:, hi, :], Pt[:, 0, h, :], AF.Relu, reads=[("Pt", h), ("nethr", tt)], writes=[("Wr", hi)], bias=nethr[:, tt, h:h + 1])
                    _act(P, Ws[:, hi, :], Pt[:, 0, h, :], AF.Sign, reads=[("Pt", h), ("nethr", tt)], writes=[("Ws", hi)], bias=nethr[:, tt, h:h + 1])

            def pe_h(u):
                eb, tt = units[u]
                for kc in range(8):
                    _mm(P, self.bank(0), xT[:, kc, tt * 128:(tt + 1) * 128], utb[:, eb % 2, kc, :], start=(kc == 0), stop=(kc == 7),
                        reads=xk + [("utb", eb % 2)], writes=[("ps", 0)])

            def pe_wsum(u):
                eb, tt = units[u]
                wb = 2 + u % 2
                for h in range(5):
                    _mm(P, self.bank(wb), self.ident[:], Wh[:, 0, h, :], start=(h == 0), stop=False,
                        reads=[("Wh", h), "ident"], writes=[("ps", wb)])
                for hi in range(3):
                    _mm(P, self.bank(wb), self.ident[:], Wr[:, hi, :], start=False, stop=False,
                        reads=[("Wr", hi), "ident"], writes=[("ps", wb)])
                for hi in range(3):
                    _mm(P, self.bank(wb), Dg[:, tt, hi, :], Ws[:, hi, :], start=False, stop=(hi == 2),
                        reads=[("Ws", hi), ("Dg", tt)], writes=[("ps", wb)])

            def act_gelu(u):
                _act(P, gl[:, u % 2, :], self.bank(0), AF.Gelu, reads=[("ps", 0)], writes=[("gl", u % 2)])

            def dve_g(u):
                eb, tt = units[u]
                wb = 2 + u % 2
                _stt(P, G[:, u % 2, :], self.bank(wb), cvec[:, tt:tt + 1], gl[:, u % 2, :], ALU.add, ALU.mult,
                     reads=[("gl", u % 2), ("ps", wb), ("cvec", tt)], writes=[("G", u % 2)])

            def pe_t(u):
                for c in range(4):
                    _mm(P, self.bank(1, c * 128, (c + 1) * 128), G[:, u % 2, c * 128:(c + 1) * 128], self.ident[:],
                        reads=[("G", u % 2), "ident"], writes=[("ps", 1)])

            def act_gt(u):
                _cp(P, 'scalar', GT[:, u % 2], self.bank(1).rearrange("p (a b) -> p a b", a=4), reads=[("ps", 1)], writes=[("GT", u % 2)])

            def pe_v(u):
                eb, tt = units[u]
                for c in range(4):
                    for hf in range(2):
                        ob = 4 + tt * 2 + hf
                        _mm(P, self.bank(ob), GT[:, u % 2, c, :], vbb[:, eb % 3, c, hf * 512:(hf + 1) * 512],
                            start=(eb == 0 and c == 0), stop=(eb == 31 and c == 3),
                            reads=[("GT", u % 2), ("vbb", eb % 3)], writes=[("ps", ob)])

            load_w(0)
            p1_pool(0)
            for i in range(nu + 2):
                if i < nu:
                    eb, tt = units[i]
                    if tt == 0 and eb + 1 < 32:
                        load_w(eb + 1)
                if i < nu:
                    p2_act(i)
                if i - 2 >= 0:
                    pe_t(i - 2)
                    act_gt(i - 2)
                if i < nu:
                    pe_h(i)
                    act_gelu(i)
                    p2_dve(i)
                if i + 1 < nu:
                    p1_pool(i + 1)
                if i - 2 >= 0:
                    pe_v(i - 2)
                if i < nu:
                    pe_wsum(i)
                if i - 1 >= 0 and i - 1 < nu:
                    dve_g(i - 1)
            for tt, t in enumerate(grp):
                ob = 4 + tt * 2
                _tt(P, 'vector', zt[:], self.psum[:, ob * 512:(ob + 2) * 512], gc[:], ALU.mult, reads=[("ps", ob), ("ps", ob + 1), "gc"], writes=["modtmp"])
                _stt(P, zt[:], hb[:, tt, :], ALPHA, zt[:], ALU.mult, ALU.add, reads=["modtmp", ("hb", tt)], writes=["modtmp"])
                self.layernorm(st, zt[:], hn[:, 0, :], lng[:], lnb[:], "modtmp", "hn", ["lng", "lnb"], "peer%d" % layer)
                if layer == 0:
                    P.dma('gpsimd', self.rows(dst, t), hn[:, 0, :], reads=["hn"], writes=["dst"])
                else:
                    i = t[1] * LT + t[2]
                    P.dma('gpsimd', dst[i * 128:(i + 1) * 128], hn[:, 0, :], reads=["hn"], writes=["dst"])
        self.barrier()


K.peer_prep = _peer_prep
K.stage_peer = _stage_peer


def _stage_mla_proj(self):
    P, nc = self.P, self.nc
    wdn_d = self.inp("mla_w_down", [1, D, 448])
    qg_d = self.inp("mla_q_norm_g", [1, 256])
    kvg_d = self.inp("mla_kv_norm_g", [1, 128])
    wuq_d = self.inp("mla_w_uq", [1, 256, 1536])
    wukv_d = self.inp("mla_w_ukv", [1, 128, 2048])
    cos_d = self.inp("cos", [SEQ, 32])
    sin_d = self.inp("sin", [SEQ, 32])
    NT = len(self.batches) * (CT + LT)
    src = self.dr["h2_d"]
    Qn_d = self.dram("Qn_d", [NT, 128, 8, 128], BF16)
    Qr_d = self.dram("Qr_d", [NT, 64, 8, 128], BF16)
    Kn_d = self.dram("Kn_d", [NT, 128, 8, 128], BF16)
    Kr_d = self.dram("Kr_d", [NT, 64, 128], BF16)
    Vt_d = self.dram("Vt_d", [NT, 128, 1024], BF16)
    with ExitStack() as st:
        sb = lambda n, s, d: self.sbuf(st, n, s, d)
        wdn = sb("wdn", [128, 8, 448], BF16)
        wuq = sb("wuq", [128, 2, 1536], BF16)
        wukv = sb("wukv", [128, 2048], BF16)
        st2 = ExitStack()
        self.load_cast(st2, [wdn[:, kc, :] for kc in range(8)],
                       [wdn_d[0].rearrange("(kc p) n -> p kc n", p=128)[:, kc, :] for kc in range(8)], 448, "wdn")
        self.load_cast(st2, [wuq[:, kc, :] for kc in range(2)],
                       [wuq_d[0].rearrange("(kc p) n -> p kc n", p=128)[:, kc, :] for kc in range(2)], 1536, "wuq")
        self.load_cast(st2, [wukv[:, :]], [wukv_d[0]], 2048, "wukv")
        self.barrier()
        st2.close()
        qg = sb("qg", [128, 256], F32)
        kvg = sb("kvg", [128, 128], F32)
        self.bcast_load(qg[:], qg_d[0], "qg")
        self.bcast_load(kvg[:], kvg_d[0], "kvg")
        sc1 = sb("sc1", [128, 1024], F32)
        sh = sb("sh", [128, 1024], F32)
        tmp = sb("modtmp", [128, 1024], F32)
        hb = sb("hb", [128, 2, 1024], F32)
        xb = sb("xb", [128, 1024], BF16)
        xT = sb("xT", [128, 8, 128], BF16)
        dn = sb("dn", [128, 448], F32)
        sq = sb("sq", [128, 256], F32)
        ss = sb("ss", [128, 4], F32)
        cqn = sb("cqn", [128, 384], BF16)
        cT = sb("cT", [128, 3, 128], BF16)
        cs = sb("cs", [128, 2, 32], F32)
        q32 = sb("q32", [128, 8, 192], F32)
        r1 = sb("r1", [128, 8, 2, 16], F32)
        r2 = sb("r2", [128, 8, 2, 16], F32)
        qb = sb("qb", [128, 8, 192], BF16)
        kb = sb("kb", [128, 8, 128], BF16)
        krb = sb("krb", [128, 64], BF16)
        vb = sb("vb", [128, 8, 128], BF16)
        qnT = sb("qnT", [128, 8, 128], BF16)
        qrT = sb("qrT", [64, 8, 128], BF16)
        knT = sb("knT", [128, 8, 128], BF16)
        krT = sb("krT", [64, 128], BF16)
        curm = None

        def rope(xv, nh, outv, kx):
            x4 = xv.rearrange("p h (a two f) -> p h a two f", a=2, two=2)
            o4 = outv.rearrange("p h (a two f) -> p h a two f", a=2, two=2)
            cosb = cs[:, 0, :].rearrange("p (a f) -> p a f", a=2).unsqueeze(1).to_broadcast([128, nh, 2, 16])
            sinb = cs[:, 1, :].rearrange("p (a f) -> p a f", a=2).unsqueeze(1).to_broadcast([128, nh, 2, 16])
            x1 = x4[:, :, :, 0, :]
            x2 = x4[:, :, :, 1, :]
            a1 = r1[:, 0:nh]
            a2 = r2[:, 0:nh]
            _tt(P, 'vector', a1, x1, cosb, ALU.mult, reads=kx + ["cs"], writes=["r1"])
            _tt(P, 'vector', a2, x2, sinb, ALU.mult, reads=kx + ["cs"], writes=["r2"])
            _tt(P, 'vector', o4[:, :, :, 0, :], a1, a2, ALU.subtract, reads=["r1", "r2"], writes=["ropeo"])
            _tt(P, 'vector', a1, x1, sinb, ALU.mult, reads=kx + ["cs", "ropeo"], writes=["r1"])
            _tt(P, 'vector', a2, x2, cosb, ALU.mult, reads=kx + ["cs", "ropeo"], writes=["r2"])
            _tt(P, 'vector', o4[:, :, :, 1, :], a1, a2, ALU.add, reads=["r1", "r2"], writes=["ropeo"])

        for n, t in enumerate(self.tiles(True)):
            par = n % 2
            lat = t[0] == 'l'
            ti = self.tid(t)
            P.dma('sync', hb[:, par, :], self.rows(src, t), writes=[("hb", par)])
            if lat:
                P.dma('sync', cs[:, 0, :], cos_d[t[2] * 128:(t[2] + 1) * 128, :], writes=["cs"])
                P.dma('sync', cs[:, 1, :], sin_d[t[2] * 128:(t[2] + 1) * 128, :], writes=["cs2"])
            mr = self.modrow(t)
            if mr != curm:
                curm = mr
                self.load_mod(1, mr, 0, sh[:], "sh")
                self.load_mod(1, mr, 1, sc1[:], "sc1", plus1=True)
            self.modulate_T(hb[:, par, :], sc1[:], sh[:], xb[:], xT, par, ["sc1", "sh"], 0, tmp[:])
            for kc in range(8):
                _mm(P, self.bank(2, 0, 448), xT[:, kc, :], wdn[:, kc, :], start=(kc == 0), stop=(kc == 7), reads=["xT", "wdn"], writes=[("ps", 2)])
            _cp(P, 'scalar', dn[:], self.bank(2, 0, 448), reads=[("ps", 2)], writes=["dn"])
            if DBG == 1:
                continue
            _act(P, sq[:, 0:256], dn[:, 0:256], AF.Square, reads=["dn"], writes=["sq", "ss"], accum_out=ss[:, 0:1])
            _act(P, sq[:, 0:128], dn[:, 256:384], AF.Square, reads=["dn"], writes=["sq", "ss"], accum_out=ss[:, 1:2])
            _ts(P, 'vector', ss[:, 2:3], ss[:, 0:1], 1.0 / 256.0, RMS_EPS, ALU.mult, ALU.add, reads=["ss"], writes=["ss2"])
            _ts(P, 'vector', ss[:, 3:4], ss[:, 1:2], 1.0 / 128.0, RMS_EPS, ALU.mult, ALU.add, reads=["ss"], writes=["ss2"])
            _act(P, ss[:, 2:4], ss[:, 2:4], AF.Sqrt, reads=["ss2"], writes=["ss2"])
            P.op('vector', lambda e: e.reciprocal(out=ss[:, 2:4], in_=ss[:, 2:4]), reads=["ss2"], writes=["ss2"])
            _stt(P, cqn[:, 0:256], dn[:, 0:256], ss[:, 2:3], qg[:], ALU.mult, ALU.mult, reads=["dn", "ss2", "qg"], writes=["cqn"])
            _stt(P, cqn[:, 256:384], dn[:, 256:384], ss[:, 3:4], kvg[:], ALU.mult, ALU.mult, reads=["dn", "ss2", "kvg"], writes=["cqn"])
            for j in range(3):
                _mm(P, self.bank(3, j * 128, (j + 1) * 128), cqn[:, j * 128:(j + 1) * 128], self.ident[:], reads=["cqn", "ident"], writes=[("ps", 3)])
            _cp(P, 'vector', cT[:], self.bank(3, 0, 384).rearrange("p (a b) -> p a b", a=3), reads=[("ps", 3)], writes=["cT"])
            for nb in range(4):
                _mm(P, self.bank(4 + nb), cT[:, 2, :], wukv[:, nb * 512:(nb + 1) * 512], reads=["cT", "wukv"], writes=[("ps", 4 + nb)])
            kvv = self.psum[:, 2048:4096].rearrange("p (h c) -> p h c", h=8)
            _cp(P, 'scalar', kb[:], kvv[:, :, 0:128], reads=[("ps", 4 + i) for i in range(4)], writes=["kb"])
            _cp(P, 'vector', vb[:], kvv[:, :, 128:256], reads=[("ps", 4 + i) for i in range(4)], writes=["vb"])
            P.dma('gpsimd', Vt_d[ti], vb[:].rearrange("p h c -> p (h c)"), reads=["vb"], writes=["Vt_d"])
            if DBG == 2:
                continue
            if lat:
                rope(dn[:, 384:448].unsqueeze(1), 1, krb[:].unsqueeze(1), ["dn", "cs2"])
            else:
                _cp(P, 'vector', krb[:], dn[:, 384:448], reads=["dn"], writes=["ropeo"])
            if DBG == 3:
                continue
            for h in range(8):
                bk = h // 4
                _mm(P, self.bank(bk, (h % 4) * 128, (h % 4 + 1) * 128), kb[:, h, :], self.ident[:], reads=["kb", "ident"], writes=[("ps", bk)])
            _cp(P, 'scalar', knT[:, 0:4, :], self.bank(0).rearrange("p (a b) -> p a b", a=4), reads=[("ps", 0)], writes=["knT"])
            _cp(P, 'vector', knT[:, 4:8, :], self.bank(1).rearrange("p (a b) -> p a b", a=4), reads=[("ps", 1)], writes=["knT"])
            _mm(P, self.psum[0:64, 2 * 512:2 * 512 + 128], krb[:], self.ident[:], reads=["ropeo", "ident"], writes=[("ps", 2)])
            _cp(P, 'scalar', krT[:], self.psum[0:64, 2 * 512:2 * 512 + 128], reads=[("ps", 2)], writes=["krT"])
            P.dma('gpsimd', Kn_d[ti], knT[:], reads=["knT"], writes=["Kn_d"])
            P.dma('gpsimd', Kr_d[ti], krT[:], reads=["krT"], writes=["Kr_d"])
            if not lat or DBG == 4:
                continue
            for nb in range(3):
                for kc in range(2):
                    _mm(P, self.bank(4 + nb), cT[:, kc, :], wuq[:, kc, nb * 512:(nb + 1) * 512], start=(kc == 0), stop=(kc == 1),
                        reads=["cT", "wuq"], writes=[("ps", 4 + nb)])
            _cp(P, 'scalar', q32[:].rearrange("p h c -> p (h c)"), self.psum[:, 2048:2048 + 1536], reads=[("ps", 4 + i) for i in range(3)], writes=["q32"])
            _cp(P, 'gpsimd', qb[:, :, 0:128], q32[:, :, 0:128], reads=["q32"], writes=["qb"])
            rope(q32[:, :, 128:192], 8, qb[:, :, 128:192], ["q32", "cs2", "qb"])
            for h in range(8):
                bk = h // 4
                _mm(P, self.bank(bk, (h % 4) * 128, (h % 4 + 1) * 128), qb[:, h, 0:128], self.ident[:], reads=["qb", "ropeo", "ident"], writes=[("ps", bk)])
            _cp(P, 'scalar', qnT[:, 0:4, :], self.bank(0).rearrange("p (a b) -> p a b", a=4), reads=[("ps", 0)], writes=["qnT"])
            _cp(P, 'vector', qnT[:, 4:8, :], self.bank(1).rearrange("p (a b) -> p a b", a=4), reads=[("ps", 1)], writes=["qnT"])
            for h in range(8):
                bk = 2 + h // 4
                _mm(P, self.psum[0:64, bk * 512 + (h % 4) * 128: bk * 512 + (h % 4 + 1) * 128], qb[:, h, 128:192], self.ident[:],
                    reads=["qb", "ropeo", "ident"], writes=[("ps", bk)])
            _cp(P, 'scalar', qrT[:, 0:4, :], self.psum[0:64, 1024:1536].rearrange("p (a b) -> p a b", a=4), reads=[("ps", 2)], writes=["qrT"])
            _cp(P, 'vector', qrT[:, 4:8, :], self.psum[0:64, 1536:2048].rearrange("p (a b) -> p a b", a=4), reads=[("ps", 3)], writes=["qrT"])
            P.dma('gpsimd', Qn_d[ti], qnT[:], reads=["qnT"], writes=["Qn_d"])
            P.dma('gpsimd', Qr_d[ti], qrT[:], reads=["qrT"], writes=["Qr_d"])
        self.barrier()


def _stage_mla_attn(self):
    P, nc = self.P, self.nc
    wo_d = self.inp("mla_w_out", [1, D, D])
    ln_g = self.inp("ln_tm_g", [2, D])
    ln_b = self.inp("ln_tm_b", [2, D])
    R = len(self.batches) * (CT + LT) * 128
    src = self.dr["h2_d"]
    h3_d = self.dram("h3_d", [R, D], F32)
    Qn_d, Qr_d, Kn_d, Kr_d, Vt_d = [self.dr[k] for k in ("Qn_d", "Qr_d", "Kn_d", "Kr_d", "Vt_d")]
    NK = (CT + LT) * 128
    SCALE = 192.0 ** -0.5
    with ExitStack() as st:
        sb = lambda n, s, d: self.sbuf(st, n, s, d)
        wob = sb("wob", [128, 8, 1024], BF16)
        st2 = ExitStack()
        self.load_cast(st2, [wob[:, kc, :] for kc in range(8)],
                       [wo_d[0].rearrange("(kc p) n -> p kc n", p=128)[:, kc, :] for kc in range(8)], 1024, "wob")
        self.barrier()
        st2.close()
        lng = sb("lng", [128, 1024], F32)
        lnb = sb("lnb", [128, 1024], F32)
        gt = sb("gt", [128, 1024], F32)
        self.bcast_load(lng[:], ln_g[1], "lng")
        self.bcast_load(lnb[:], ln_b[1], "lnb")
        KT = sb("KT", [128, 8, NK], BF16)
        KrT = sb("KrT", [64, NK], BF16)
        V = sb("V", [128, CT + LT, 1024], BF16)
        Qn = sb("Qn", [128, 2, 8, 128], BF16)
        Qr = sb("Qr", [64, 2, 8, 128], BF16)
        ht = sb("ht", [128, 2, 1024], F32)
        Pm = sb("Pm", [128, NK], BF16)
        PT = sb("PT", [128, CT + LT, 128], BF16)
        mx = sb("mx", [128, 4], F32)
        oat = sb("oat", [128, 1024], BF16)
        oT = sb("oT", [128, 8, 128], BF16)
        zt = sb("zt", [128, 1024], F32)
        hn = sb("hn", [128, 2, 1024], F32)
        nq = 0
        nkb = CT + LT
        for b in self.batches:
            base = (b - self.batches[0]) * (CT + LT)
            for j in range(nkb):
                P.dma('sync', KT[:, :, j * 128:(j + 1) * 128], Kn_d[base + j], writes=[("KT", j)])
                P.dma('sync', KrT[:, j * 128:(j + 1) * 128], Kr_d[base + j], writes=[("KrT", j)])
                P.dma('sync', V[:, j, :], Vt_d[base + j], writes=[("V", j)])
            kk = [("KT", j) for j in range(nkb)] + [("KrT", j) for j in range(nkb)]
            vk = [("V", j) for j in range(nkb)]
            self.load_mod(1, b, 2, gt[:], "gt")
            for jq in range(LT):
                t = ('l', b, jq)
                ti = self.tid(t)
                par = nq % 2
                nq += 1
                P.dma('sync', Qn[:, par], Qn_d[ti], writes=[("Qn", par)])
                P.dma('sync', Qr[:, par], Qr_d[ti], writes=[("Qr", par)])
                P.dma('sync', ht[:, par, :], self.rows(src, t), writes=[("ht", par)])
                for h in range(8):
                    for nb in range(5):
                        c0 = nb * 512
                        c1 = min(NK, c0 + 512)
                        _mm(P, self.bank(nb, 0, c1 - c0), Qn[:, par, h, :], KT[:, h, c0:c1], start=True, stop=False,
                            reads=[("Qn", par)] + kk, writes=[("ps", nb)])
                        _mm(P, self.bank(nb, 0, c1 - c0), Qr[:, par, h, :], KrT[:, c0:c1], start=False, stop=True,
                            reads=[("Qr", par)] + kk, writes=[("ps", nb)])
                    sk = [("ps", i) for i in range(5)]
                    P.op('vector', lambda e: e.tensor_reduce(out=mx[:, 0:1], in_=self.psum[:, 0:NK], axis=AX.X, op=ALU.max), reads=sk, writes=["mx"])
                    _ts(P, 'vector', mx[:, 1:2], mx[:, 0:1], -SCALE, None, ALU.mult, reads=["mx"], writes=["mx1"])
                    _act(P, Pm[:], self.psum[:, 0:NK], AF.Exp, reads=sk + ["mx1"], writes=["Pm", "mx2"], bias=mx[:, 1:2], scale=SCALE, accum_out=mx[:, 2:3])
                    P.op('vector', lambda e: e.reciprocal(out=mx[:, 3:4], in_=mx[:, 2:3]), reads=["mx2"], writes=["mx3"])
                    for r0 in range(0, nkb, 8):
                        nr = min(8, nkb - r0)
                        for i in range(nr):
                            bk = 5 + i // 4
                            _mm(P, self.bank(bk, (i % 4) * 128, (i % 4 + 1) * 128), Pm[:, (r0 + i) * 128:(r0 + i + 1) * 128], self.ident[:],
                                reads=["Pm", "ident"], writes=[("ps", bk)])
                        n0 = min(4, nr)
                        _cp(P, 'vector', PT[:, r0:r0 + n0, :], self.bank(5, 0, n0 * 128).rearrange("p (a b) -> p a b", a=n0), reads=[("ps", 5)], writes=["PT"])
                        if nr > 4:
                            _cp(P, 'scalar', PT[:, r0 + 4:r0 + nr, :], self.bank(6, 0, (nr - 4) * 128).rearrange("p (a b) -> p a b", a=nr - 4),
                                reads=[("ps", 6)], writes=["PT"])
                    for kbi in range(nkb):
                        _mm(P, self.bank(7, 0, 128), PT[:, kbi, :], V[:, kbi, h * 128:(h + 1) * 128], start=(kbi == 0), stop=(kbi == nkb - 1),
                            reads=["PT"] + vk, writes=[("ps", 7)])
                    _ts(P, 'vector', oat[:, h * 128:(h + 1) * 128], self.bank(7, 0, 128), mx[:, 3:4], None, ALU.mult, reads=[("ps", 7), "mx3"], writes=["oat"])
                for kc in range(8):
                    bk = 5 + kc // 4
                    _mm(P, self.bank(bk, (kc % 4) * 128, (kc % 4 + 1) * 128), oat[:, kc * 128:(kc + 1) * 128], self.ident[:],
                        reads=["oat", "ident"], writes=[("ps", bk)])
                _cp(P, 'scalar', oT[:, 0:4, :], self.bank(5).rearrange("p (a b) -> p a b", a=4), reads=[("ps", 5)], writes=["oT"])
                _cp(P, 'vector', oT[:, 4:8, :], self.bank(6).rearrange("p (a b) -> p a b", a=4), reads=[("ps", 6)], writes=["oT"])
                for hf in range(2):
                    for kc in range(8):
                        _mm(P, self.bank(5 + hf), oT[:, kc, :], wob[:, kc, hf * 512:(hf + 1) * 512], start=(kc == 0), stop=(kc == 7),
                            reads=["oT", "wob"], writes=[("ps", 5 + hf)])
                _tt(P, 'vector', zt[:], self.psum[:, 5 * 512:7 * 512], gt[:], ALU.mult, reads=[("ps", 5), ("ps", 6), "gt"], writes=["zt"])
                _stt(P, zt[:], ht[:, par, :], ALPHA, zt[:], ALU.mult, ALU.add, reads=["zt", ("ht", par)], writes=["zt"])
                self.layernorm(st, zt[:], hn[:, par, :], lng[:], lnb[:], "zt", ("hn", par), ["lng", "lnb"], "mla")
                P.dma('gpsimd', self.rows(h3_d, t), hn[:, par, :], reads=[("hn", par)], writes=["h3_d"])
        self.barrier()


K.stage_mla_proj = _stage_mla_proj
K.stage_mla_attn = _stage_mla_attn


ALL_STAGES = ['prep', 'gla_proj', 'gla_fwd', 'gla_bwd', 'peer0', 'mla_proj', 'mla_attn', 'peer1']
_CACHE = {}


def kernel(**inputs):
    NB = 4
    k = K(NB, ALL_STAGES)
    nc = k.build()
    consts = host_consts()
    f32 = lambda a: np.ascontiguousarray(np.asarray(a, dtype=np.float32))
    x = f32(inputs['x'])
    ctx = f32(inputs['ctx'])
    c = f32(inputs['c'])
    c_ctx = f32(inputs['c_ctx'])
    shared = {}
    for name in k.dr:
        if name in inputs and name not in ('x', 'ctx'):
            shared[name] = f32(inputs[name])
        elif name in consts:
            shared[name] = consts[name]
    maps = []
    for core in range(NCORES):
        m = dict(shared)
        m['x'] = np.ascontiguousarray(x[core * NB:(core + 1) * NB])
        m['ctx'] = np.ascontiguousarray(ctx[core * NB:(core + 1) * NB])
        cv = np.concatenate([c[core * NB:(core + 1) * NB], c_ctx[None]], 0)
        m['cT'] = np.ascontiguousarray(cv.T.reshape(8, 128, 5).transpose(1, 0, 2))
        maps.append(m)
    res = run_bass_kernel_spmd(nc, maps, core_ids=list(range(NCORES)))
    outs = [np.asarray(res.results[i]['out']).reshape(NB, SEQ, D) for i in range(NCORES)]
    return np.concatenate(outs, axis=0).astype(np.float32)
```
